# Optimizing a Trainium2 kernel written in Bass

```python
import math
import jax, jax.numpy as jnp
from jax import lax
import numpy as np


D_MODEL = 4096
BATCH = 4
SEQ = 4096
DEPTH = 1

MIX_WIDTH = D_MODEL
CONV_WIDTH = MIX_WIDTH // 2
CONV_K = 3
CONV_GROUPS = 16
CONV_GROUP_DIM = CONV_WIDTH // CONV_GROUPS
ATTN_WIDTH = MIX_WIDTH - CONV_WIDTH
DIFF_HEAD_DIM = 64
N_HEADS = ATTN_WIDTH // (2 * DIFF_HEAD_DIM)
V_HEAD_DIM = 2 * DIFF_HEAD_DIM
ROPE_THETA = 10000.0
Q_BLOCK = 128
N_GROUPS = 8
EXPERTS_PER_GROUP = 8
N_EXPERTS = N_GROUPS * EXPERTS_PER_GROUP
TOP_K = 2
D_EXPERT = D_MODEL // 4
MOE_BLOCK = 256
NORM_EPS = 1e-6
SUBLN_EPS = 1e-5
IN_COLS = 3 * CONV_WIDTH + 3 * ATTN_WIDTH

kernel_name = "hybrid_shortconv_diffattn_hmoe"


def rms_norm(x, w, eps):
    xf = x.astype(jnp.float32)
    y = xf * lax.rsqrt(jnp.mean(xf * xf, axis=-1, keepdims=True) + eps)
    return (y * w.astype(jnp.float32)).astype(x.dtype)


def rope(t, positions):
    dh = t.shape[-1]
    half = dh // 2
    inv_freq = 1.0 / (ROPE_THETA ** (jnp.arange(half, dtype=jnp.float32) * 2.0 / dh))
    ang = positions.astype(jnp.float32)[:, :, None] * inv_freq
    cos = jnp.cos(ang)[:, :, None, None, :]
    sin = jnp.sin(ang)[:, :, None, None, :]
    t1 = t[..., :half].astype(jnp.float32)
    t2 = t[..., half:].astype(jnp.float32)
    return jnp.concatenate([t1 * cos - t2 * sin, t2 * cos + t1 * sin], axis=-1).astype(t.dtype)


def short_conv_branch(b_gate, c_gate, u, conv_w, conv_norm_w):
    bsz, s, c = u.shape
    z = c_gate * u
    kern = conv_w[:, None, :]
    zc = lax.conv_general_dilated(z, kern, window_strides=(1,), padding=[(CONV_K - 1, 0)],
                                  dimension_numbers=('NWC', 'WIO', 'NWC'),
                                  feature_group_count=c)
    y = (b_gate * zc).astype(jnp.float32).reshape(bsz, s, CONV_GROUPS, CONV_GROUP_DIM)
    y = y * lax.rsqrt(jnp.mean(y * y, axis=-1, keepdims=True) + NORM_EPS)
    y = y.reshape(bsz, s, c) * conv_norm_w.astype(jnp.float32)
    return y.astype(u.dtype)


def diff_attention_branch(q, k, v, positions, lam_q1, lam_k1, lam_q2, lam_k2, subln_w, lam_init):
    bsz, s, _ = q.shape
    q = rope(q.reshape(bsz, s, N_HEADS, 2, DIFF_HEAD_DIM), positions)
    k = rope(k.reshape(bsz, s, N_HEADS, 2, DIFF_HEAD_DIM), positions)
    v = v.reshape(bsz, s, N_HEADS, V_HEAD_DIM).transpose(0, 2, 1, 3)
    q = q.transpose(0, 2, 3, 1, 4)
    k = k.transpose(0, 2, 3, 1, 4)
    f32 = jnp.float32
    lam = (jnp.exp(jnp.sum(lam_q1.astype(f32) * lam_k1.astype(f32)))
           - jnp.exp(jnp.sum(lam_q2.astype(f32) * lam_k2.astype(f32))) + lam_init)
    scale = DIFF_HEAD_DIM ** -0.5
    nq = s // Q_BLOCK
    qb = q.reshape(bsz, N_HEADS, 2, nq, Q_BLOCK, DIFF_HEAD_DIM).transpose(3, 0, 1, 2, 4, 5)
    kpos = jnp.arange(s)

    def block(args):
        qblk, i = args
        sc = jnp.einsum('bhcqd,bhckd->bhcqk', qblk, k).astype(f32) * scale
        qpos = i * Q_BLOCK + jnp.arange(Q_BLOCK)
        sc = jnp.where(kpos[None, :] <= qpos[:, None], sc, -jnp.inf)
        p = jax.nn.softmax(sc, axis=-1)
        p = p[:, :, 0] - lam * p[:, :, 1]
        return jnp.einsum('bhqk,bhkd->bhqd', p.astype(v.dtype), v)

    o = lax.map(block, (qb, jnp.arange(nq)))
    o = o.transpose(1, 0, 3, 2, 4).reshape(bsz, s, N_HEADS, V_HEAD_DIM)
    of = o.astype(f32)
    of = of * lax.rsqrt(jnp.mean(of * of, axis=-1, keepdims=True) + SUBLN_EPS)
    of = of * subln_w.astype(f32) * (1.0 - lam_init)
    return of.reshape(bsz, s, ATTN_WIDTH).astype(q.dtype)


def hier_moe(h, w_rg, b_rg, w_re, b_re, w1, w3, w2):
    bsz, s, d = h.shape
    n = bsz * s
    f32 = jnp.float32
    xt = h.reshape(n, d)
    g_logits = (xt @ w_rg).astype(f32) + b_rg.astype(f32)
    g_prob = jax.nn.softmax(g_logits, axis=-1)
    g_sel = jnp.argmax(g_logits, axis=-1).astype(jnp.int32)
    g_gate = jnp.take_along_axis(g_prob, g_sel[:, None], axis=-1)[:, 0]
    e_logits = ((xt @ w_re).astype(f32) + b_re.astype(f32)).reshape(n, N_GROUPS, EXPERTS_PER_GROUP)
    e_in = jnp.take_along_axis(e_logits, g_sel[:, None, None], axis=1)[:, 0]
    top_v, top_i = lax.top_k(e_in, TOP_K)
    gates = g_gate[:, None] * jax.nn.softmax(top_v, axis=-1)
    expert = g_sel[:, None] * EXPERTS_PER_GROUP + top_i.astype(jnp.int32)
    a = n * TOP_K
    a_e = expert.reshape(a)
    a_tok = jnp.repeat(jnp.arange(n, dtype=jnp.int32), TOP_K)
    a_w = gates.reshape(a)
    order = jnp.argsort(a_e)
    s_e, s_tok, s_w = a_e[order], a_tok[order], a_w[order]
    counts = jnp.bincount(a_e, length=N_EXPERTS).astype(jnp.int32)
    starts = jnp.cumsum(counts) - counts
    pcounts = (counts + MOE_BLOCK - 1) // MOE_BLOCK * MOE_BLOCK
    pends = jnp.cumsum(pcounts)
    pstarts = pends - pcounts
    dest = pstarts[s_e] + (jnp.arange(a, dtype=jnp.int32) - starts[s_e])
    nb = (a + MOE_BLOCK - 1) // MOE_BLOCK + N_EXPERTS
    p_rows = nb * MOE_BLOCK
    buf_tok = jnp.full((p_rows,), n, jnp.int32).at[dest].set(s_tok)
    buf_w = jnp.zeros((p_rows,), f32).at[dest].set(s_w)
    blk_e = jnp.minimum(jnp.searchsorted(pends, jnp.arange(nb, dtype=jnp.int32) * MOE_BLOCK, side='right'),
                        N_EXPERTS - 1)
    xpad = jnp.concatenate([xt, jnp.zeros((1, d), xt.dtype)], axis=0)
    xb = xpad[buf_tok].reshape(nb, MOE_BLOCK, d)
    wb = buf_w.reshape(nb, MOE_BLOCK)

    def expert_block(args):
        xblk, wblk, e = args
        hid = jax.nn.silu(xblk @ w1[e]) * (xblk @ w3[e])
        return (hid @ w2[e]).astype(f32) * wblk[:, None]

    yb = lax.map(expert_block, (xb, wb, blk_e)).reshape(p_rows, d)
    out = jnp.zeros((n + 1, d), f32).at[buf_tok].add(yb)[:n]
    return out.astype(h.dtype).reshape(bsz, s, d)


def setup_inputs(seed: int = 0) -> dict:
    key = jax.random.key(seed)
    ks = jax.random.split(key, 20)

    def nrm(k, shape, scale):
        return jax.random.normal(k, shape, jnp.float32) * scale

    return {
        'x': nrm(ks[0], (BATCH, SEQ, D_MODEL), 1.0),
        'positions': jnp.broadcast_to(jnp.arange(SEQ, dtype=jnp.int32), (BATCH, SEQ)),
        'ln1_w': 1.0 + nrm(ks[1], (DEPTH, D_MODEL), 0.02),
        'w_in': nrm(ks[2], (DEPTH, D_MODEL, IN_COLS), D_MODEL ** -0.5),
        'conv_w': nrm(ks[3], (DEPTH, CONV_K, CONV_WIDTH), CONV_K ** -0.5),
        'conv_norm_w': 1.0 + nrm(ks[4], (DEPTH, CONV_WIDTH), 0.02),
        'lam_q1': nrm(ks[5], (DEPTH, DIFF_HEAD_DIM), 0.1),
        'lam_k1': nrm(ks[6], (DEPTH, DIFF_HEAD_DIM), 0.1),
        'lam_q2': nrm(ks[7], (DEPTH, DIFF_HEAD_DIM), 0.1),
        'lam_k2': nrm(ks[8], (DEPTH, DIFF_HEAD_DIM), 0.1),
        'subln_w': 1.0 + nrm(ks[9], (DEPTH, V_HEAD_DIM), 0.02),
        'w_out': nrm(ks[10], (DEPTH, MIX_WIDTH, D_MODEL), MIX_WIDTH ** -0.5),
        'ln2_w': 1.0 + nrm(ks[11], (DEPTH, D_MODEL), 0.02),
        'w_router_group': nrm(ks[12], (DEPTH, D_MODEL, N_GROUPS), D_MODEL ** -0.5),
        'b_router_group': nrm(ks[13], (DEPTH, N_GROUPS), 0.01),
        'w_router_expert': nrm(ks[14], (DEPTH, D_MODEL, N_EXPERTS), D_MODEL ** -0.5),
        'b_router_expert': nrm(ks[15], (DEPTH, N_EXPERTS), 0.01),
        'w1': nrm(ks[16], (DEPTH, N_EXPERTS, D_MODEL, D_EXPERT), D_MODEL ** -0.5),
        'w3': nrm(ks[17], (DEPTH, N_EXPERTS, D_MODEL, D_EXPERT), D_MODEL ** -0.5),
        'w2': nrm(ks[18], (DEPTH, N_EXPERTS, D_EXPERT, D_MODEL), D_EXPERT ** -0.5),
        'lnf_w': 1.0 + nrm(ks[19], (D_MODEL,), 0.02),
    }


def reference(x, positions, ln1_w, w_in, conv_w, conv_norm_w, lam_q1, lam_k1, lam_q2, lam_k2,
              subln_w, w_out, ln2_w, w_router_group, b_router_group, w_router_expert,
              b_router_expert, w1, w3, w2, lnf_w):
    h = x
    cuts = [CONV_WIDTH, 2 * CONV_WIDTH, 3 * CONV_WIDTH,
            3 * CONV_WIDTH + ATTN_WIDTH, 3 * CONV_WIDTH + 2 * ATTN_WIDTH]
    for l in range(DEPTH):
        lam_init = 0.8 - 0.6 * math.exp(-0.3 * l)
        hn = rms_norm(h, ln1_w[l], NORM_EPS)
        proj = hn @ w_in[l]
        b_gate, c_gate, u, q, k, v = jnp.split(proj, cuts, axis=-1)
        conv_out = short_conv_branch(b_gate, c_gate, u, conv_w[l], conv_norm_w[l])
        attn_out = diff_attention_branch(q, k, v, positions, lam_q1[l], lam_k1[l], lam_q2[l],
                                         lam_k2[l], subln_w[l], lam_init)
        h = h + jnp.concatenate([conv_out, attn_out], axis=-1) @ w_out[l]
        h = h + hier_moe(rms_norm(h, ln2_w[l], NORM_EPS), w_router_group[l], b_router_group[l],
                         w_router_expert[l], b_router_expert[l], w1[l], w3[l], w2[l])
    return rms_norm(h, lnf_w, NORM_EPS)
```

```python
import math
from contextlib import ExitStack
import numpy as np
import concourse.bass as bass
import concourse.mybir as mybir
from concourse.bass_utils import run_bass_kernel_spmd

F32 = mybir.dt.float32
BF16 = mybir.dt.bfloat16
I32 = mybir.dt.int32
ALU = mybir.AluOpType
AF = mybir.ActivationFunctionType
AX = mybir.AxisListType

PE, ACT, DVE, POOL, SP = 0, 1, 2, 3, 4


class Ev:
    __slots__ = ("kind", "eng", "seq", "sem", "semid", "val", "clock", "dclock")


class Buf:
    __slots__ = ("name", "w", "r", "excl")

    def __init__(self, name="", excl=False):
        self.name = name
        self.w = None
        self.r = []
        self.excl = excl


class Eng:
    def __init__(self, idx, b, sem):
        self.idx = idx
        self.b = b
        self.sem = sem
        self.count = 0
        self.known = [0] * 5
        self.kd = {}


class Prog:
    def __init__(self, nc, es, n_dma_sems=16):
        self.nc = nc
        builders = [nc.tensor, nc.scalar, nc.vector, nc.gpsimd, nc.sync]
        self.E = []
        for i, b in enumerate(builders):
            sem = es.enter_context(nc.semaphore("esem%d" % i))
            self.E.append(Eng(i, b, sem))
        self.dsems = {}
        for q in (SP, POOL, ACT):
            ring = []
            for j in range(n_dma_sems):
                sem = es.enter_context(nc.semaphore("dsem%d_%d" % (q, j)))
                ring.append([sem, 0, None])
            self.dsems[q] = [ring, 0]

    def _merge(self, E, ev):
        k = E.known
        c = ev.clock
        for i in range(5):
            if c[i] > k[i]:
                k[i] = c[i]
        kd = E.kd
        for s, v in ev.dclock.items():
            if kd.get(s, 0) < v:
                kd[s] = v

    def _wait(self, E, ev):
        if ev.kind == 0:
            if ev.eng is E and E.idx == PE:
                return
            if E.known[ev.eng.idx] >= ev.seq:
                return
            E.b.wait_ge(ev.eng.sem, ev.seq)
            E.known[ev.eng.idx] = ev.seq
            self._merge(E, ev)
        else:
            if E.kd.get(ev.semid, 0) >= ev.val:
                return
            E.b.wait_ge(ev.sem, ev.val)
            E.kd[ev.semid] = ev.val
            self._merge(E, ev)

    def _deps(self, E, reads, writes):
        for b in reads:
            if b.w is not None:
                self._wait(E, b.w)
        for b in writes:
            if b.w is not None:
                self._wait(E, b.w)
            for r in b.r:
                self._wait(E, r)

    def _post(self, ev, reads, writes):
        for b in reads:
            b.r.append(ev)
        for b in writes:
            b.w = ev
            b.r = []

    limit = None
    nops = 0

    def op(self, e, fn, reads=(), writes=()):
        self.nops += 1
        if self.limit is not None and self.nops > self.limit:
            return None
        E = self.E[e]
        writes = list(writes) + [b for b in reads if b.excl]
        reads = [b for b in reads if not b.excl]
        self._deps(E, reads, writes)
        inst = fn(E.b)
        E.count += 1
        inst.then_inc(E.sem, 1)
        ev = Ev()
        ev.kind = 0
        ev.eng = E
        ev.seq = E.count
        ev.clock = list(E.known)
        ev.dclock = dict(E.kd)
        self._post(ev, reads, writes)
        return ev

    def dma(self, q, fn, reads=(), writes=()):
        self.nops += 1
        if self.limit is not None and self.nops > self.limit:
            return None
        E = self.E[q]
        self._deps(E, reads, writes)
        ringinfo = self.dsems[q]
        ring, pos = ringinfo
        slot = ring[pos % len(ring)]
        ringinfo[1] = pos + 1
        if slot[2] is not None:
            self._wait(E, slot[2])
        inst = fn(E.b)
        slot[1] += 1
        inst.then_inc(slot[0], 16)
        ev = Ev()
        ev.kind = 1
        ev.sem = slot[0]
        ev.semid = (q, pos % len(ring))
        ev.val = 16 * slot[1]
        ev.clock = list(E.known)
        ev.dclock = dict(E.kd)
        slot[2] = ev
        self._post(ev, reads, writes)
        return ev

    def _all_events(self):
        evs = []
        for E in self.E:
            if E.count > 0:
                ev = Ev()
                ev.kind = 0
                ev.eng = E
                ev.seq = E.count
                ev.clock = [0] * 5
                ev.dclock = {}
                evs.append(ev)
        for q, (ring, pos) in self.dsems.items():
            for slot in ring:
                if slot[2] is not None:
                    evs.append(slot[2])
        return evs

    def barrier(self):
        evs = self._all_events()
        for E in self.E:
            for ev in evs:
                self._wait(E, ev)

    def wait_all(self, e):
        E = self.E[e]
        for ev in self._all_events():
            self._wait(E, ev)


class _Stop(Exception):
    pass


class Cfg:
    def __init__(self, D=4096, S=4096, B=4, NEXP=64, NGRP=8):
        self.D, self.S, self.B = D, S, B
        self.T = S // 2
        self.NC = 2 * B
        self.KC = D // 128
        self.CW = D // 2
        self.AW = D // 2
        self.NCB = self.CW // 128
        self.NH = self.AW // 128
        self.INC = 3 * self.CW + 3 * self.AW
        self.DE = D // 4
        self.HC = self.DE // 128
        self.NEXP = NEXP
        self.NGRP = NGRP
        self.EPG = NEXP // NGRP
        self.NT = self.T // 128
        self.NG = self.T // 512
        self.CAP = 128
        self.NR = NGRP + NEXP


MAGIC = 12582912.0
TWO_PI = 2.0 * math.pi
CW1 = 6.28125
CW2 = float(np.float32(TWO_PI - CW1))
NEG = -30000.0

C_ID, C_RP, C_TRI, C_US, C_ONES = 0, 128, 256, 384, 512
C_INVF, C_PIDX, C_EBASE = 640, 641, 642


def make_consts(cfg):
    n = C_EBASE + cfg.NEXP
    c = np.zeros((128, n), np.float32)
    c[:, C_ID:C_ID + 128] = np.eye(128, dtype=np.float32)
    rp = np.zeros((128, 128), np.float32)
    for m in range(128):
        if m % 64 < 32:
            rp[m + 32, m] = -1.0
        else:
            rp[m - 32, m] = 1.0
    c[:, C_RP:C_RP + 128] = rp
    k = np.arange(128)[:, None]
    q = np.arange(128)[None, :]
    c[:, C_TRI:C_TRI + 128] = (q >= k).astype(np.float32)
    c[:, C_US:C_US + 128] = (k < q).astype(np.float32)
    c[:, C_ONES:C_ONES + 128] = 1.0
    half = 32
    inv = (1.0 / (10000.0 ** (np.arange(half, dtype=np.float32) * 2.0 / 64.0))).astype(np.float32)
    c[:, C_INVF] = inv[np.arange(128) % 32]
    c[:, C_PIDX] = np.arange(128, dtype=np.float32)
    c[:, C_EBASE:C_EBASE + cfg.NEXP] = (np.arange(cfg.NEXP, dtype=np.float32) * cfg.CAP)[None, :]
    return c


def build_nc(cfg, lam_init=0.2, dbg=False, stop=None):
    D, T, KC, NCB, NH, DE, HC, NEXP, NT, NG = (cfg.D, cfg.T, cfg.KC, cfg.NCB, cfg.NH, cfg.DE,
                                              cfg.HC, cfg.NEXP, cfg.NT, cfg.NG)
    CW, AW, INC, NR, CAP, NGRP, EPG = cfg.CW, cfg.AW, cfg.INC, cfg.NR, cfg.CAP, cfg.NGRP, cfg.EPG
    TT = 2 * T
    NTT = TT // 128
    NCONST = C_EBASE + NEXP
    nc = bass.Bass("TRN2", target_bir_lowering=False)

    def din(name, shape, dt=F32):
        return nc.dram_tensor(name, list(shape), dt, kind="ExternalInput").ap()

    def dscr(name, shape, dt):
        return nc.dram_tensor(name, list(shape), dt, kind=("ExternalOutput" if dbg else "Internal")).ap()

    xo = din("xo", [T, D])
    xp = din("xp", [T, D])
    pos = din("pos", [1, TT], I32)
    kb = din("kb", [128, NTT])
    ln1w = din("ln1w", [128, KC])
    w_in = din("w_in", [D, INC])
    convw = din("convw", [128, NCB * 3])
    cnw = din("cnw", [128, NCB])
    lam4 = din("lam4", [1, 256])
    subw = din("subw", [1, 128])
    w_out = din("w_out", [D, D])
    ln2w = din("ln2w", [1, D])
    lnfw = din("lnfw", [1, D])
    wr = din("wr", [D, NR])
    br = din("br", [1, NR])
    w1 = din("w1", [NEXP, D, DE])
    w3 = din("w3", [NEXP, D, DE])
    w2 = din("w2", [NEXP, DE, D])
    consts = din("consts", [128, NCONST])
    out = nc.dram_tensor("out", [T, D], F32, kind="ExternalOutput").ap()

    QT = dscr("QT", [NH, 128, T], BF16)
    KT = dscr("KT", [NH, 128, TT], BF16)
    VS = dscr("VS", [NH, 128, NTT, 128], BF16)
    MIXT = dscr("MIXT", [D, T], BF16)
    H = dscr("H", [T, D], F32)
    HN2 = dscr("HN2", [T + 128, D], BF16)
    Y = dscr("Y", [NEXP * CAP, D], F32)
    SLOT = dscr("SLOT", [NEXP * CAP, 16], I32)
    RT = dscr("RT", [T, 8], F32)

    with ExitStack() as ges:
        p = Prog(nc, ges)
        import os
        if os.environ.get('OPLIMIT'):
            p.limit = int(os.environ['OPLIMIT'])

        def sb(es, name, shape, dt=F32):
            return es.enter_context(nc.sbuf_tensor(name, list(shape), dt))

        cst = sb(ges, "cst", [128, NCONST])
        identb = sb(ges, "identb", [128, 128], BF16)
        rpermf = cst[:, C_RP:C_RP + 128]
        trib = sb(ges, "trib", [128, 128], BF16)
        usb = sb(ges, "usb", [128, 128], BF16)
        onesb = sb(ges, "onesb", [128, 128], BF16)
        meanb = sb(ges, "meanb", [128, 128], BF16)
        epsn = sb(ges, "epsn", [128, 1])
        epss = sb(ges, "epss", [128, 1])
        nlam = sb(ges, "nlam", [128, 1])
        dests = sb(ges, "dests", [128, NT, 2], I32)
        gates = sb(ges, "gates", [128, NT, 2])
        CST, IDB, TRIB, USB, ONESB, MEANB, EPS, NLAM, DESTS, GATES = [Buf() for _ in range(10)]
        banks = [ges.enter_context(nc.psum_tensor("bank%d" % i, [128, 512], F32)) for i in range(8)]
        BK = [Buf("bank%d" % i, excl=True) for i in range(8)]

        p.dma(SP, lambda q: q.dma_start(out=cst[:], in_=consts), writes=[CST])
        p.op(DVE, lambda e: e.tensor_copy(out=identb[:], in_=cst[:, C_ID:C_ID + 128]), reads=[CST], writes=[IDB])
        p.op(DVE, lambda e: e.tensor_copy(out=trib[:], in_=cst[:, C_TRI:C_TRI + 128]), reads=[CST], writes=[TRIB])
        p.op(DVE, lambda e: e.tensor_copy(out=usb[:], in_=cst[:, C_US:C_US + 128]), reads=[CST], writes=[USB])
        p.op(DVE, lambda e: e.tensor_copy(out=onesb[:], in_=cst[:, C_ONES:C_ONES + 128]), reads=[CST], writes=[ONESB])
        p.op(DVE, lambda e: e.tensor_scalar(out=meanb[:], in0=cst[:, C_ONES:C_ONES + 128], scalar1=1.0 / 128.0,
                                            scalar2=None, op0=ALU.mult), reads=[CST], writes=[MEANB])
        p.op(DVE, lambda e: e.memset(epsn[:], 1e-6), writes=[EPS])
        p.op(DVE, lambda e: e.memset(epss[:], 1e-5), writes=[EPS])

        try:
            with ExitStack() as es:
                xt = [sb(es, "xt%d" % i, [128, D]) for i in range(2)]
                XT = [Buf() for _ in range(2)]
                hnb = sb(es, "hnb", [128, D], BF16)
                HNB = Buf()
                hnT = [sb(es, "hnT%d" % i, [128, KC, 512], BF16) for i in range(2)]
                HNT = [Buf() for _ in range(2)]
                halo_hn = sb(es, "halo_hn", [128, KC, 2], BF16)
                HALOHN = Buf()
                NWR = 3
                wring = [sb(es, "wr%d" % i, [128, KC, 256], BF16) for i in range(NWR)]
                WR = [Buf() for _ in range(NWR)]
                ln1t = sb(es, "ln1t", [128, KC])
                cwt = sb(es, "cwt", [128, NCB * 3])
                cnt = sb(es, "cnt", [128, NCB])
                kbt = sb(es, "kbt", [128, NTT])
                SMALL = Buf()
                st = [sb(es, "st%d" % i, [128, 1]) for i in range(3)]
                ST = [Buf() for _ in range(3)]
                posi = sb(es, "posi", [128, 512], I32)
                ang = sb(es, "ang", [128, 512])
                kk = sb(es, "kk", [128, 512])
                cosT = sb(es, "cosT", [128, 512])
                sinT = sb(es, "sinT", [128, 512])
                POSI, ANG, KKB, COS, SIN = [Buf() for _ in range(5)]
                zhalo = sb(es, "zhalo", [128, NCB, 2])
                ZH = Buf()
                sd = sb(es, "sd", [128, 512])
                SD = Buf()
                cout = [sb(es, "cout%d" % i, [128, 512], BF16) for i in range(2)]
                COUT = [Buf() for _ in range(2)]
                t1 = sb(es, "t1", [128, 512])
                T1 = Buf()
                t2 = sb(es, "t2", [128, 512])
                T2 = Buf()
                qk = [sb(es, "qk%d" % i, [128, 512], BF16) for i in range(2)]
                QK = [Buf() for _ in range(2)]
                vtok = [sb(es, "vtok%d" % i, [128, 4, 128], BF16) for i in range(2)]
                VTOK = [Buf() for _ in range(2)]
                lamt = sb(es, "lamt", [128, 256])
                lamp = sb(es, "lamp", [128, 128])
                LAM = Buf()

                p.dma(SP, lambda q: q.dma_start(out=ln1t[:], in_=ln1w), writes=[SMALL])
                p.dma(SP, lambda q: q.dma_start(out=cwt[:], in_=convw), writes=[SMALL])
                p.dma(SP, lambda q: q.dma_start(out=cnt[:], in_=cnw), writes=[SMALL])
                p.dma(SP, lambda q: q.dma_start(out=kbt[:], in_=kb), writes=[SMALL])
                p.op(DVE, lambda e: e.memset(zhalo[:], 0.0), writes=[ZH])
                p.op(DVE, lambda e: e.memset(halo_hn[:], 0.0), writes=[HALOHN])
                p.dma(SP, lambda q: q.dma_start(out=lamt[:], in_=lam4.partition_broadcast(128)), writes=[LAM])
                p.op(DVE, lambda e: e.tensor_tensor(out=lamp[:, 0:64], in0=lamt[:, 0:64], in1=lamt[:, 64:128], op=ALU.mult), reads=[LAM], writes=[LAM])
                p.op(DVE, lambda e: e.tensor_tensor(out=lamp[:, 64:128], in0=lamt[:, 128:192], in1=lamt[:, 192:256], op=ALU.mult), reads=[LAM], writes=[LAM])
                p.op(DVE, lambda e: e.tensor_reduce(out=st[0][:], in_=lamp[:, 0:64], axis=AX.X, op=ALU.add), reads=[LAM], writes=[ST[0]])
                p.op(DVE, lambda e: e.tensor_reduce(out=st[1][:], in_=lamp[:, 64:128], axis=AX.X, op=ALU.add), reads=[LAM], writes=[ST[1]])
                p.op(ACT, lambda e: e.activation(out=st[0][:], in_=st[0][:], func=AF.Exp), reads=[ST[0]], writes=[ST[0]])
                p.op(ACT, lambda e: e.activation(out=st[1][:], in_=st[1][:], func=AF.Exp), reads=[ST[1]], writes=[ST[1]])
                p.op(DVE, lambda e: e.tensor_tensor(out=st[2][:], in0=st[1][:], in1=st[0][:], op=ALU.subtract), reads=[ST[0], ST[1]], writes=[ST[2]])
                p.op(DVE, lambda e: e.tensor_scalar(out=nlam[:], in0=st[2][:], scalar1=-float(lam_init), scalar2=None, op0=ALU.add),
                     reads=[ST[2]], writes=[NLAM])

                yb2 = [sb(es, "yb2_%d" % i, [128, 512]) for i in range(2)]
                ysq2 = [sb(es, "ysq2_%d" % i, [128, 512], BF16) for i in range(2)]
                qf2 = [sb(es, "qf2_%d" % i, [128, 512]) for i in range(2)]
                vb2 = [sb(es, "vb2_%d" % i, [128, 512], BF16) for i in range(2)]
                YB2 = [Buf() for _ in range(2)]
                YSQ2 = [Buf() for _ in range(2)]
                QF2 = [Buf() for _ in range(2)]
                VB2 = [Buf() for _ in range(2)]
                cs2 = [sb(es, "cs2_%d" % i, [128, 512]) for i in range(2)]
                zt2 = [sb(es, "zt2_%d" % i, [128, 514]) for i in range(2)]
                acc2 = [sb(es, "acc2_%d" % i, [128, 512]) for i in range(2)]
                CS2 = [Buf() for _ in range(2)]
                ZT2 = [Buf() for _ in range(2)]
                ACC2 = [Buf() for _ in range(2)]
                ucount = [0]
                wcount = [0]
                pcount = [0]
                xcount = [0]

                def load_wblk(col0):
                    s = wcount[0] % NWR
                    wcount[0] += 1
                    src = w_in[:, col0:col0 + 256].rearrange("(k p) n -> p k n", p=128)
                    p.dma(POOL, lambda q: q.dma_start(out=wring[s][:], in_=src), writes=[WR[s]])
                    return s

                def proj(s, sub=0, ncols=512, col_lo=0, rhs_t=None, bank=None):
                    if bank is None:
                        bank = pcount[0] % 4
                        pcount[0] += 1
                    src = hnT[curg[0]] if rhs_t is None else rhs_t

                    def f(e):
                        r = None
                        for kc in range(KC):
                            r = e.matmul(banks[bank][:, 0:ncols], wring[s][:, kc, sub * 128:(sub + 1) * 128], src[:, kc, col_lo:col_lo + ncols],
                                         start=(kc == 0), stop=(kc == KC - 1))
                        return r
                    p.op(PE, f, reads=[WR[s], HNT[curg[0]] if rhs_t is None else HALOHN], writes=[BK[bank]])
                    return bank

                def issue_x(n_):
                    gi_, i_ = n_ // 4, n_ % 4
                    src_ = xp if gi_ < NG else xo
                    g_ = gi_ if gi_ < NG else gi_ - NG
                    r0_ = g_ * 512 + i_ * 128
                    p.dma(SP, lambda q: q.dma_start(out=xt[n_ % 2][:], in_=src_[r0_:r0_ + 128, :]), writes=[XT[n_ % 2]])

                def prep_tile(gi, i):
                    is_prev = gi < NG
                    g = gi if is_prev else gi - NG
                    xsrc = xp if is_prev else xo
                    hT = hnT[gi % 2]
                    HT_ = HNT[gi % 2]
                    n_ = gi * 4 + i
                    xs = n_ % 2
                    if n_ == 0:
                        issue_x(0)
                    if n_ + 1 < 8 * NG:
                        issue_x(n_ + 1)
                    p.op(ACT, lambda e: e.activation(out=hnb[:], in_=xt[xs][:], func=AF.Square, accum_out=st[0][:]),
                         reads=[XT[xs]], writes=[HNB, ST[0]])
                    p.op(ACT, lambda e: e.activation(out=st[1][:], in_=st[0][:], func=AF.Sqrt, bias=epsn[:], scale=1.0 / D),
                         reads=[ST[0], EPS], writes=[ST[1]])
                    p.op(DVE, lambda e: e.reciprocal(out=st[2][:], in_=st[1][:]), reads=[ST[1]], writes=[ST[2]])
                    p.op(DVE, lambda e: e.tensor_scalar(out=hnb[:], in0=xt[xs][:], scalar1=st[2][:, 0:1], scalar2=None, op0=ALU.mult),
                         reads=[XT[xs], ST[2]], writes=[HNB])
                    for k4 in range(0, KC, 4):
                        bkid = 4 if (k4 // 4) % 2 == 0 else 6
                        tb = banks[bkid][:].bitcast(BF16)

                        def ftr0(e):
                            r = None
                            for kc in range(k4, k4 + 4):
                                r = e.transpose(tb[:, (kc - k4) * 128:(kc - k4 + 1) * 128], hnb[:, kc * 128:(kc + 1) * 128], identb[:])
                            return r
                        p.op(PE, ftr0, reads=[HNB, IDB], writes=[BK[bkid]])
                        for kc in range(k4, k4 + 4):
                            off = (kc - k4) * 128
                            if bkid == 6:
                                p.op(ACT, lambda e: e.activation(out=hT[:, kc, i * 128:(i + 1) * 128], in_=tb[:, off:off + 128],
                                                                 func=AF.Copy, scale=ln1t[:, kc:kc + 1]),
                                     reads=[BK[bkid], SMALL], writes=[HT_])
                            else:
                                p.op(DVE, lambda e: e.tensor_scalar(out=hT[:, kc, i * 128:(i + 1) * 128], in0=tb[:, off:off + 128],
                                                                    scalar1=ln1t[:, kc:kc + 1], scalar2=None, op0=ALU.mult),
                                     reads=[BK[bkid], SMALL], writes=[HT_])
                    if gi == NG - 1 and i == 3:
                        p.op(DVE, lambda e: e.tensor_copy(out=halo_hn[:], in_=hT[:, :, 510:512]), reads=[HT_], writes=[HALOHN])

                pending = []
                units = [0]

                def tick():
                    units[0] += 1
                    while pending and units[0] >= pending[0][0]:
                        _, a = pending.pop(0)
                        prep_tile(*a)

                for i_ in range(4):
                    prep_tile(0, i_)
                curg = [0]
                for gi in range(2 * NG):
                    is_prev = gi < NG
                    g = gi if is_prev else gi - NG
                    tok0 = gi * 512
                    curg[0] = gi % 2
                    n_units = (2 * NH) if is_prev else (NCB + 3 * NH)
                    units[0] = 0
                    if gi + 1 < 2 * NG:
                        pending[:] = [(max(1, (n_units * (k_ + 1)) // 5), (gi + 1, k_)) for k_ in range(4)]
                    p.dma(SP, lambda q: q.dma_start(out=posi[:], in_=pos[0:1, tok0:tok0 + 512].partition_broadcast(128)), writes=[POSI])
                    p.op(DVE, lambda e: e.tensor_copy(out=ang[:], in_=posi[:]), reads=[POSI], writes=[ANG])
                    p.op(DVE, lambda e: e.tensor_scalar(out=ang[:], in0=ang[:], scalar1=cst[:, C_INVF:C_INVF + 1], scalar2=None, op0=ALU.mult),
                         reads=[ANG, CST], writes=[ANG])
                    p.op(DVE, lambda e: e.tensor_scalar(out=kk[:], in0=ang[:], scalar1=1.0 / TWO_PI, scalar2=MAGIC, op0=ALU.mult, op1=ALU.add),
                         reads=[ANG], writes=[KKB])
                    p.op(DVE, lambda e: e.tensor_scalar(out=kk[:], in0=kk[:], scalar1=MAGIC, scalar2=None, op0=ALU.subtract),
                         reads=[KKB], writes=[KKB])
                    p.op(DVE, lambda e: e.scalar_tensor_tensor(out=ang[:], in0=kk[:], scalar=-CW1, in1=ang[:], op0=ALU.mult, op1=ALU.add),
                         reads=[KKB, ANG], writes=[ANG])
                    p.op(DVE, lambda e: e.scalar_tensor_tensor(out=ang[:], in0=kk[:], scalar=-CW2, in1=ang[:], op0=ALU.mult, op1=ALU.add),
                         reads=[KKB, ANG], writes=[ANG])
                    p.op(ACT, lambda e: e.activation(out=sinT[:], in_=ang[:], func=AF.Sin), reads=[ANG], writes=[SIN])
                    p.op(DVE, lambda e: e.tensor_scalar(out=ang[:], in0=ang[:], scalar1=math.pi / 2, scalar2=None, op0=ALU.add),
                         reads=[ANG], writes=[ANG])
                    p.op(DVE, lambda e: e.tensor_scalar(out=kk[:], in0=ang[:], scalar1=math.pi, scalar2=-TWO_PI, op0=ALU.is_gt, op1=ALU.mult),
                         reads=[ANG], writes=[KKB])
                    p.op(DVE, lambda e: e.tensor_tensor(out=ang[:], in0=ang[:], in1=kk[:], op=ALU.add), reads=[ANG, KKB], writes=[ANG])
                    p.op(ACT, lambda e: e.activation(out=cosT[:], in_=ang[:], func=AF.Sin), reads=[ANG], writes=[COS])

                    deferred_pe = []

                    def flush_deferred():
                        while deferred_pe:
                            deferred_pe.pop(0)()

                    if not is_prev:
                        for j0 in range(0, NCB, 2):
                            sC = load_wblk(CW + j0 * 128)
                            sU = load_wblk(2 * CW + j0 * 128)
                            sB = load_wblk(j0 * 128)
                            bCs = []
                            for sub in range(2):
                                bC = proj(sC, sub)
                                flush_deferred()
                                p.op(ACT, lambda e: e.activation(out=cs2[sub][:], in_=banks[bC][:], func=AF.Copy), reads=[BK[bC]], writes=[CS2[sub]])
                            for sub in range(2):
                                j = j0 + sub
                                zt_ = zt2[sub]
                                ZT_ = ZT2[sub]
                                acc_ = acc2[sub]
                                ACC_ = ACC2[sub]
                                bU = proj(sU, sub)
                                if g == 0:
                                    proj(sC, sub, ncols=2, rhs_t=halo_hn, bank=6)
                                    p.op(ACT, lambda e: e.activation(out=zt_[:, 0:2], in_=banks[6][:, 0:2], func=AF.Copy), reads=[BK[6]], writes=[ZT_])
                                    proj(sU, sub, ncols=2, rhs_t=halo_hn, bank=6)
                                    p.op(DVE, lambda e: e.tensor_tensor(out=zt_[:, 0:2], in0=banks[6][:, 0:2], in1=zt_[:, 0:2], op=ALU.mult),
                                         reads=[BK[6], ZT_], writes=[ZT_])
                                else:
                                    p.op(DVE, lambda e: e.tensor_copy(out=zt_[:, 0:2], in_=zhalo[:, j, :]), reads=[ZH], writes=[ZT_])
                                p.op(DVE, lambda e: e.tensor_tensor(out=zt_[:, 2:514], in0=banks[bU][:], in1=cs2[sub][:], op=ALU.mult),
                                     reads=[BK[bU], CS2[sub]], writes=[ZT_])
                                p.op(DVE, lambda e: e.tensor_copy(out=zhalo[:, j, :], in_=zt_[:, 512:514]), reads=[ZT_], writes=[ZH])
                                p.op(DVE, lambda e: e.tensor_scalar(out=acc_[:], in0=zt_[:, 2:514], scalar1=cwt[:, j * 3 + 2:j * 3 + 3], scalar2=None, op0=ALU.mult),
                                     reads=[ZT_, SMALL], writes=[ACC_])
                                p.op(DVE, lambda e: e.scalar_tensor_tensor(out=acc_[:], in0=zt_[:, 1:513], scalar=cwt[:, j * 3 + 1:j * 3 + 2], in1=acc_[:],
                                                                            op0=ALU.mult, op1=ALU.add), reads=[ZT_, SMALL, ACC_], writes=[ACC_])
                                p.op(DVE, lambda e: e.scalar_tensor_tensor(out=acc_[:], in0=zt_[:, 0:512], scalar=cwt[:, j * 3:j * 3 + 1], in1=acc_[:],
                                                                            op0=ALU.mult, op1=ALU.add), reads=[ZT_, SMALL, ACC_], writes=[ACC_])
                            for sub in range(2):
                                j = j0 + sub
                                bB = proj(sB, sub)
                                pb = ucount[0] % 2
                                ucount[0] += 1
                                p.op(DVE, lambda e: e.tensor_tensor(out=yb2[pb][:], in0=banks[bB][:], in1=acc2[sub][:], op=ALU.mult),
                                     reads=[BK[bB], ACC2[sub]], writes=[YB2[pb]])
                                p.op(ACT, lambda e: e.activation(out=ysq2[pb][:], in_=yb2[pb][:], func=AF.Square), reads=[YB2[pb]], writes=[YSQ2[pb]])

                                def rest_conv(j=j, pb=pb, g=g):
                                    p.op(PE, lambda e: e.matmul(banks[6][:], meanb[:], ysq2[pb][:], start=True, stop=True), reads=[MEANB, YSQ2[pb]], writes=[BK[6]])
                                    p.op(ACT, lambda e: e.activation(out=sd[:], in_=banks[6][:], func=AF.Sqrt, bias=epsn[:], scale=1.0),
                                         reads=[BK[6], EPS], writes=[SD])
                                    p.op(DVE, lambda e: e.reciprocal(out=sd[:], in_=sd[:]), reads=[SD], writes=[SD])
                                    co = j % 2
                                    p.op(DVE, lambda e: e.scalar_tensor_tensor(out=cout[co][:], in0=yb2[pb][:], scalar=cnt[:, j:j + 1], in1=sd[:],
                                                                               op0=ALU.mult, op1=ALU.mult), reads=[YB2[pb], SMALL, SD], writes=[COUT[co]])
                                    p.dma(SP, lambda q: q.dma_start(out=MIXT[j * 128:(j + 1) * 128, g * 512:(g + 1) * 512], in_=cout[co][:]),
                                          reads=[COUT[co]])
                                flush_deferred()
                                deferred_pe.append(rest_conv)
                                tick()
                    for h0, which, sub in [(h0_, w_, sub_) for h0_ in range(0, NH, 2) for w_ in ((1,) if is_prev else (0, 1)) for sub_ in range(2)]:
                        if True:
                            h = h0 + sub
                            if sub == 0:
                                s_qk = load_wblk(3 * CW + which * AW + h0 * 128)
                            s = s_qk
                            bq = proj(s, sub)
                            flush_deferred()
                            pb = ucount[0] % 2
                            ucount[0] += 1
                            p.op(ACT, lambda e: e.activation(out=qf2[pb][:], in_=banks[bq][:], func=AF.Copy), reads=[BK[bq]], writes=[QF2[pb]])

                            def rest_qk(h=h, which=which, pb=pb, g=g, tok0=tok0):
                                p.op(PE, lambda e: e.matmul(banks[5][:], rpermf, qf2[pb][:], start=True, stop=True), reads=[CST, QF2[pb]], writes=[BK[5]])
                                p.op(DVE, lambda e: e.tensor_tensor(out=t1[:], in0=qf2[pb][:], in1=cosT[:], op=ALU.mult), reads=[QF2[pb], COS], writes=[T1])
                                p.op(DVE, lambda e: e.tensor_tensor(out=t2[:], in0=banks[5][:], in1=sinT[:], op=ALU.mult), reads=[BK[5], SIN], writes=[T2])
                                o = (h * 2 + which) % 2
                                p.op(DVE, lambda e: e.tensor_tensor(out=qk[o][:], in0=t1[:], in1=t2[:], op=ALU.add), reads=[T1, T2], writes=[QK[o]])
                                if which == 0:
                                    p.dma(SP, lambda q: q.dma_start(out=QT[h, :, g * 512:(g + 1) * 512], in_=qk[o][:]), reads=[QK[o]])
                                else:
                                    p.dma(SP, lambda q: q.dma_start(out=KT[h, :, tok0:tok0 + 512], in_=qk[o][:]), reads=[QK[o]])
                            deferred_pe.append(rest_qk)
                            tick()
                    for h in range(NH):
                        if h % 2 == 0:
                            s_v = load_wblk(3 * CW + 2 * AW + h * 128)
                        s = s_v
                        bv = proj(s, h % 2)
                        flush_deferred()
                        pb = ucount[0] % 2
                        ucount[0] += 1
                        p.op(ACT, lambda e: e.activation(out=vb2[pb][:], in_=banks[bv][:], func=AF.Copy), reads=[BK[bv]], writes=[VB2[pb]])

                        def rest_v(h=h, pb=pb, tok0=tok0):
                            tb7 = banks[7][:].bitcast(BF16)

                            def ftr(e):
                                r = None
                                for i in range(4):
                                    r = e.transpose(tb7[:, i * 128:(i + 1) * 128], vb2[pb][:, i * 128:(i + 1) * 128], identb[:])
                                return r
                            p.op(PE, ftr, reads=[VB2[pb], IDB], writes=[BK[7]])
                            vo = h % 2
                            p.op(DVE, lambda e: e.tensor_copy(out=vtok[vo][:].rearrange("p a b -> p (a b)"), in_=tb7[:, 0:512]),
                                 reads=[BK[7]], writes=[VTOK[vo]])
                            t0 = tok0 // 128
                            p.dma(SP, lambda q: q.dma_start(out=VS[h, :, t0:t0 + 4, :], in_=vtok[vo][:]), reads=[VTOK[vo]])
                        deferred_pe.append(rest_v)
                        tick()
                    flush_deferred()
                    while pending:
                        _, a_ = pending.pop(0)
                        prep_tile(*a_)
                p.barrier()
            if stop == 'A':
                raise _Stop()

            with ExitStack() as es:
                qT = [sb(es, "qT%d" % i, [128, T], BF16) for i in range(2)]
                kT = [[sb(es, "kT%d_%d" % (i, c_), [128, TT], BF16) for c_ in range(2)] for i in range(2)]
                vS = [sb(es, "vS%d" % i, [128, NTT, 130], BF16) for i in range(2)]
                HB = [Buf() for _ in range(2)]
                NER = 8
                et = [sb(es, "et%d" % i, [128, 512], BF16) for i in range(NER)]
                ET = [Buf() for _ in range(NER)]
                kbt = sb(es, "kbt2", [128, NTT])
                subt = sb(es, "subt", [128, 128])
                SM = Buf()
                rz = sb(es, "rz", [128, 4])
                RZ = Buf()
                o1 = sb(es, "o1", [128, 128])
                O1 = Buf()
                o2 = sb(es, "o2", [128, 128])
                O2 = Buf()
                osq = sb(es, "osq", [128, 128])
                OSQ = Buf()
                of_ = sb(es, "of", [128, 128], BF16)
                OF = Buf()
                ao = [sb(es, "ao%d" % i, [128, 512], BF16) for i in range(2)]
                AO = [Buf() for _ in range(2)]
                p.dma(SP, lambda q: q.dma_start(out=kbt[:], in_=kb), writes=[SM])
                p.dma(SP, lambda q: q.dma_start(out=subt[:], in_=subw.partition_broadcast(128)), writes=[SM])
                p.op(DVE, lambda e: e.tensor_scalar(out=subt[:], in0=subt[:], scalar1=float(1.0 - lam_init), scalar2=None, op0=ALU.mult),
                     reads=[SM], writes=[SM])
                for i in range(2):
                    p.op(DVE, lambda e: e.memset(kT[i][0][64:128, :], 0.0), writes=[HB[i]])
                    p.op(DVE, lambda e: e.memset(kT[i][1][0:64, :], 0.0), writes=[HB[i]])
                    p.op(DVE, lambda e: e.memset(vS[i][:, :, 128:129], 1.0), writes=[HB[i]])
                    p.op(DVE, lambda e: e.memset(vS[i][:, :, 129:130], 0.0), writes=[HB[i]])
                ecount = [0]
                aocount = [0]
                nhalf_t = sb(es, "nhalf_t", [128, 1])
                ob = [[sb(es, "ob%d_%d" % (j_, i_), [128, 386]) for i_ in range(4)] for j_ in range(2)]
                OB = [[Buf() for i_ in range(4)] for j_ in range(2)]
                rzs = [sb(es, "rz%d" % i_, [128, 4]) for i_ in range(4)]
                RZS = [Buf() for _ in range(4)]
                o1s = [sb(es, "o1_%d" % i_, [128, 128]) for i_ in range(4)]
                o2s = [sb(es, "o2_%d" % i_, [128, 128]) for i_ in range(4)]
                osqs = [sb(es, "osq_%d" % i_, [128, 128]) for i_ in range(4)]
                ofs = [sb(es, "of_%d" % i_, [128, 128], BF16) for i_ in range(4)]
                O1S = [Buf() for _ in range(4)]
                O2S = [Buf() for _ in range(4)]
                OSQS = [Buf() for _ in range(4)]
                OFS = [Buf() for _ in range(4)]
                NH_ = Buf()
                p.op(DVE, lambda e: e.memset(nhalf_t[:], -0.5), writes=[NH_])
                nhalfT = sb(es, "nhalfT", [128, 512])
                p.op(DVE, lambda e: e.memset(nhalfT[:], -0.5), writes=[NH_])
                subcol = sb(es, "subcol", [128, 1])
                p.dma(SP, lambda q: q.dma_start(out=subcol[:], in_=subw.rearrange("o d -> d o")), writes=[SM])
                p.op(DVE, lambda e: e.tensor_scalar(out=subcol[:], in0=subcol[:], scalar1=float(1.0 - lam_init), scalar2=None, op0=ALU.mult),
                     reads=[SM], writes=[SM])
                eps_t = [[sb(es, "ep%d_%d" % (j_, i_), [128, 512], BF16 if i_ >= 5 else F32) for i_ in range(7)] for j_ in range(2)]
                EPS_B = [[Buf() for i_ in range(7)] for j_ in range(2)]
                qbcount = [0]
                for h in range(NH):
                    hs = h % 2
                    p.dma(SP, lambda q: q.dma_start(out=qT[hs][:], in_=QT[h]), writes=[HB[hs]])
                    p.dma(SP, lambda q: q.dma_start(out=kT[hs][0][0:64, :], in_=KT[h, 0:64, :]), writes=[HB[hs]])
                    p.dma(SP, lambda q: q.dma_start(out=kT[hs][1][64:128, :], in_=KT[h, 64:128, :]), writes=[HB[hs]])
                    p.dma(SP, lambda q: q.dma_start(out=vS[hs][:, :, 0:128], in_=VS[h]), writes=[HB[hs]])
                    for qb in range(NG):
                        nkt = NT + 4 * qb + 4

                        def geom(kt):
                            dj = kt - (NT + 4 * qb)
                            lo = 128 * dj if dj > 0 else 0
                            return dj, lo, 512 - lo

                        def emit_qk(kt, c):
                            dj, lo, n = geom(kt)
                            sbk = c + 6 * (kt % 2)
                            p.op(PE, lambda e: e.matmul(banks[sbk][:, 0:n], kT[hs][c][:, kt * 128:(kt + 1) * 128],
                                                        qT[hs][:, qb * 512 + lo:qb * 512 + 512], start=True, stop=True),
                                 reads=[HB[hs]], writes=[BK[sbk]])
                            es_ = ecount[0] % NER
                            ecount[0] += 1
                            p.op(ACT, lambda e: e.activation(out=et[es_][:, 0:n], in_=banks[sbk][:, 0:n], func=AF.Exp,
                                                             bias=kbt[:, kt:kt + 1], scale=0.125),
                                 reads=[BK[sbk], SM], writes=[ET[es_]])
                            if dj >= 0:
                                p.op(POOL, lambda e: e.tensor_tensor(out=et[es_][:, 0:128], in0=et[es_][:, 0:128], in1=trib[:], op=ALU.mult),
                                     reads=[ET[es_], TRIB], writes=[ET[es_]])
                            return es_

                        def emit_pv(kt, c, es_):
                            dj, lo, n = geom(kt)

                            def fpv(e):
                                e.matmul(banks[2 + c][:, lo:512], vS[hs][:, kt, 0:128], et[es_][:, 0:n], start=(kt == 0), stop=(kt == nkt - 1))
                                return e.matmul(banks[4 + c][:, lo:512], onesb[:], et[es_][:, 0:n], start=(kt == 0), stop=(kt == nkt - 1))
                            p.op(PE, fpv, reads=[ET[es_], HB[hs], ONESB], writes=[BK[2 + c], BK[4 + c]])
                        slots = {}
                        for kt0 in range(2):
                            for c in range(2):
                                slots[(kt0, c)] = emit_qk(kt0, c)
                        for kt in range(nkt):
                            for c in range(2):
                                emit_pv(kt, c, slots.pop((kt, c)))
                                if kt + 2 < nkt:
                                    slots[(kt + 2, c)] = emit_qk(kt + 2, c)
                        par = qbcount[0] % 2
                        qbcount[0] += 1
                        zA, zB, tA, tB, vv, osqb, aot = eps_t[par]
                        ZA, ZB, TA, TB, VV, OSQB, AOT = EPS_B[par]
                        p.op(DVE, lambda e: e.reciprocal(out=zA[:], in_=banks[4][:]), reads=[BK[4]], writes=[ZA])
                        p.op(DVE, lambda e: e.reciprocal(out=zB[:], in_=banks[5][:]), reads=[BK[5]], writes=[ZB])
                        p.op(DVE, lambda e: e.tensor_tensor(out=tA[:], in0=banks[2][:], in1=zA[:], op=ALU.mult), reads=[BK[2], ZA], writes=[TA])
                        p.op(DVE, lambda e: e.tensor_tensor(out=tB[:], in0=banks[3][:], in1=zB[:], op=ALU.mult), reads=[BK[3], ZB], writes=[TB])
                        p.op(DVE, lambda e: e.scalar_tensor_tensor(out=tA[:], in0=tB[:], scalar=nlam[:, 0:1], in1=tA[:], op0=ALU.mult, op1=ALU.add),
                             reads=[TB, NLAM, TA], writes=[TA])
                        p.op(DVE, lambda e: e.tensor_tensor(out=osqb[:], in0=tA[:], in1=tA[:], op=ALU.mult), reads=[TA], writes=[OSQB])
                        p.op(PE, lambda e: e.matmul(banks[6][:], meanb[:], osqb[:], start=True, stop=True), reads=[MEANB, OSQB], writes=[BK[6]])
                        p.op(ACT, lambda e: e.activation(out=vv[:], in_=banks[6][:], func=AF.Ln, bias=epss[:], scale=1.0), reads=[BK[6], EPS], writes=[VV])
                        p.op(ACT, lambda e: e.activation(out=vv[:], in_=vv[:], func=AF.Exp, scale=-0.5), reads=[VV], writes=[VV])
                        p.op(DVE, lambda e: e.scalar_tensor_tensor(out=aot[:], in0=tA[:], scalar=subcol[:, 0:1], in1=vv[:], op0=ALU.mult, op1=ALU.mult),
                             reads=[TA, SM, VV], writes=[AOT])
                        p.dma(SP, lambda q: q.dma_start(out=MIXT[CW + h * 128:CW + (h + 1) * 128, qb * 512:(qb + 1) * 512], in_=aot[:]),
                              reads=[AOT])
                p.barrier()
            if stop == 'C':
                raise _Stop()

            with ExitStack() as es:
                GT = 512
                NTG = GT // 128
                mixT = sb(es, "mixT", [128, KC, GT], BF16)
                MX = Buf()
                NWO = 2
                wo = [sb(es, "wo%d" % i, [128, KC, 256], BF16) for i in range(NWO)]
                WO = [Buf() for _ in range(NWO)]
                ht = [sb(es, "ht%d" % i, [128, D]) for i in range(NTG)]
                HT = [Buf() for _ in range(NTG)]
                ln2t = sb(es, "ln2t", [128, D])
                hn2 = sb(es, "hn2", [128, D])
                HN2B = Buf()
                hn2b = sb(es, "hn2b", [128, D], BF16)
                HN2BB = Buf()
                hn2T = sb(es, "hn2T", [128, KC, 128])
                HN2T = Buf()
                wrt = sb(es, "wrt", [128, KC, NR])
                brt = sb(es, "brt", [128, NR])
                SM = Buf()
                st = [sb(es, "sst%d" % i, [128, 1]) for i in range(3)]
                ST = [Buf() for _ in range(3)]
                lg = sb(es, "lg", [128, NR])
                LG = Buf()
                sc = sb(es, "sc", [128, 16])
                SC = Buf()
                ohg = sb(es, "ohg", [128, NGRP])
                exg = sb(es, "exg", [128, NGRP])
                tmp = sb(es, "tmp", [128, NEXP])
                ein = sb(es, "ein", [128, EPG])
                e2 = sb(es, "e2", [128, EPG])
                oh1 = sb(es, "oh1", [128, EPG])
                oh2 = sb(es, "oh2", [128, EPG])
                A1 = sb(es, "A1", [128, NEXP])
                A2 = sb(es, "A2", [128, NEXP])
                Ab = sb(es, "Ab", [128, NEXP], BF16)
                cum = sb(es, "cum", [128, NEXP])
                slot = sb(es, "slot", [128, NEXP])
                tokid = sb(es, "tokid", [128, 16], I32)
                tokf = sb(es, "tokf", [128, 16])
                rt = sb(es, "rtt", [128, 8])
                sinit = sb(es, "sinit", [128, 16], I32)
                RB = Buf()
                CUM = Buf()
                TOK = Buf()
                ZR = Buf()
                HN2D = Buf()
                SLOTD = Buf()
                p.dma(SP, lambda q: q.dma_start(out=ln2t[:], in_=ln2w.partition_broadcast(128)), writes=[SM])
                p.dma(SP, lambda q: q.dma_start(out=wrt[:], in_=wr.rearrange("(k p) n -> p k n", p=128)), writes=[SM])
                p.dma(SP, lambda q: q.dma_start(out=brt[:], in_=br.partition_broadcast(128)), writes=[SM])
                p.op(DVE, lambda e: e.memset(cum[:], 0.0), writes=[CUM])
                p.op(DVE, lambda e: e.memset(hn2b[:], 0.0), writes=[HN2BB])
                p.dma(SP, lambda q: q.dma_start(out=HN2[T:T + 128, :], in_=hn2b[:]), reads=[HN2BB], writes=[HN2D])
                p.op(POOL, lambda e: e.iota(sinit[:], pattern=[[0, 16]], base=T, channel_multiplier=0), writes=[TOK])
                for e_ in range(NEXP):
                    p.dma(SP, lambda q: q.dma_start(out=SLOT[e_ * CAP:(e_ + 1) * CAP, :], in_=sinit[:]), reads=[TOK], writes=[SLOTD])
                wocount = [0]
                deferred = []
                tokall = sb(es, "tokall", [128, NT, 16], I32)
                TOKA = Buf()
                for ti_ in range(NT):
                    p.op(POOL, lambda e: e.iota(tokall[:, ti_, :], pattern=[[0, 16]], base=ti_ * 128, channel_multiplier=1), writes=[TOKA])
                for gi in range(T // GT):
                    p.dma(SP, lambda q: q.dma_start(out=mixT[:], in_=MIXT[:, gi * GT:(gi + 1) * GT].rearrange("(k p) n -> p k n", p=128)),
                          writes=[MX])
                    for i in range(NTG):
                        r0 = gi * GT + i * 128
                        p.dma(SP, lambda q: q.dma_start(out=ht[i][:], in_=xo[r0:r0 + 128, :]), writes=[HT[i]])
                    for cb in range(D // 256):
                        s = wocount[0] % NWO
                        wocount[0] += 1
                        src = w_out[:, cb * 256:(cb + 1) * 256].rearrange("(k p) n -> p k n", p=128)
                        p.dma(POOL, lambda q: q.dma_start(out=wo[s][:], in_=src), writes=[WO[s]])
                        for i in range(NTG):
                            bk = i

                            def fo(e):
                                r = None
                                for kc in range(KC):
                                    r = e.matmul(banks[bk][:, 0:256], mixT[:, kc, i * 128:(i + 1) * 128], wo[s][:, kc, :],
                                                 start=(kc == 0), stop=(kc == KC - 1))
                                return r
                            p.op(PE, fo, reads=[MX, WO[s]], writes=[BK[bk]])
                            p.op(DVE, lambda e: e.tensor_tensor(out=ht[i][:, cb * 256:(cb + 1) * 256], in0=banks[bk][:, 0:256],
                                                                in1=ht[i][:, cb * 256:(cb + 1) * 256], op=ALU.add),
                                 reads=[BK[bk], HT[i]], writes=[HT[i]])
                    for i in range(NTG):
                        ti = gi * NTG + i
                        r0 = ti * 128
                        p.dma(SP, lambda q: q.dma_start(out=H[r0:r0 + 128, :], in_=ht[i][:]), reads=[HT[i]])
                        p.op(ACT, lambda e: e.activation(out=hn2b[:], in_=ht[i][:], func=AF.Square, accum_out=st[0][:]),
                             reads=[HT[i]], writes=[HN2BB, ST[0]])
                        p.op(ACT, lambda e: e.activation(out=st[1][:], in_=st[0][:], func=AF.Sqrt, bias=epsn[:], scale=1.0 / D),
                             reads=[ST[0], EPS], writes=[ST[1]])
                        p.op(DVE, lambda e: e.reciprocal(out=st[2][:], in_=st[1][:]), reads=[ST[1]], writes=[ST[2]])
                        p.op(DVE, lambda e: e.scalar_tensor_tensor(out=hn2[:], in0=ht[i][:], scalar=st[2][:, 0:1], in1=ln2t[:],
                                                                   op0=ALU.mult, op1=ALU.mult), reads=[HT[i], ST[2], SM], writes=[HN2B])
                        p.op(ACT, lambda e: e.activation(out=hn2b[:], in_=hn2[:], func=AF.Copy), reads=[HN2B], writes=[HN2BB])
                        p.dma(SP, lambda q: q.dma_start(out=HN2[r0:r0 + 128, :], in_=hn2b[:]), reads=[HN2BB], writes=[HN2D])
                        for k4 in range(0, KC, 4):
                            bkid = 4 if (k4 // 4) % 2 == 0 else 7

                            def ftr1(e):
                                r = None
                                for kc in range(k4, k4 + 4):
                                    r = e.transpose(banks[bkid][:, (kc - k4) * 128:(kc - k4 + 1) * 128], hn2[:, kc * 128:(kc + 1) * 128], cst[:, C_ID:C_ID + 128])
                                return r
                            p.op(PE, ftr1, reads=[HN2B, CST], writes=[BK[bkid]])
                            if bkid == 4:
                                p.op(ACT, lambda e: e.activation(out=hn2T[:, k4:k4 + 4, :].rearrange("p a b -> p (a b)"), in_=banks[bkid][:], func=AF.Copy),
                                     reads=[BK[bkid]], writes=[HN2T])
                            else:
                                p.op(DVE, lambda e: e.tensor_copy(out=hn2T[:, k4:k4 + 4, :].rearrange("p a b -> p (a b)"), in_=banks[bkid][:]),
                                     reads=[BK[bkid]], writes=[HN2T])

                        def frt(e):
                            r = None
                            for kc in range(KC):
                                r = e.matmul(banks[5][:, 0:NR], hn2T[:, kc, :], wrt[:, kc, :], start=(kc == 0), stop=(kc == KC - 1))
                            return r
                        p.op(PE, frt, reads=[HN2T, SM], writes=[BK[5]])
                        R = [RB]
                        dv = lambda fn, rd=(), wr_=(): p.op(DVE, fn, reads=list(rd) + R, writes=list(wr_) + R)
                        dv(lambda e: e.tensor_tensor(out=lg[:], in0=banks[5][:, 0:NR], in1=brt[:], op=ALU.add), rd=[BK[5], SM])
                        dv(lambda e: e.tensor_reduce(out=sc[:, 0:1], in_=lg[:, 0:NGRP], axis=AX.X, op=ALU.max))
                        dv(lambda e: e.tensor_scalar(out=ohg[:], in0=lg[:, 0:NGRP], scalar1=sc[:, 0:1], scalar2=None, op0=ALU.is_equal))
                        dv(lambda e: e.tensor_scalar(out=sc[:, 1:2], in0=sc[:, 0:1], scalar1=-1.0, scalar2=None, op0=ALU.mult))
                        p.op(ACT, lambda e: e.activation(out=exg[:], in_=lg[:, 0:NGRP], func=AF.Exp, bias=sc[:, 1:2], scale=1.0, accum_out=sc[:, 2:3]),
                             reads=R, writes=R)
                        dv(lambda e: e.reciprocal(out=sc[:, 3:4], in_=sc[:, 2:3]))
                        le3 = lg[:, NGRP:NR].rearrange("p (g j) -> p g j", g=NGRP)
                        tmp3 = tmp[:].rearrange("p (g j) -> p g j", g=NGRP)
                        dv(lambda e: e.tensor_tensor(out=tmp3, in0=le3, in1=ohg[:].unsqueeze(2).to_broadcast([128, NGRP, EPG]), op=ALU.mult))
                        dv(lambda e: e.tensor_reduce(out=ein[:], in_=tmp[:].rearrange("p (g j) -> p j g", g=NGRP), axis=AX.X, op=ALU.add))
                        dv(lambda e: e.tensor_reduce(out=sc[:, 4:5], in_=ein[:], axis=AX.X, op=ALU.max))
                        dv(lambda e: e.tensor_scalar(out=oh1[:], in0=ein[:], scalar1=sc[:, 4:5], scalar2=None, op0=ALU.is_equal))
                        dv(lambda e: e.scalar_tensor_tensor(out=e2[:], in0=oh1[:], scalar=-1e30, in1=ein[:], op0=ALU.mult, op1=ALU.add))
                        dv(lambda e: e.tensor_reduce(out=sc[:, 5:6], in_=e2[:], axis=AX.X, op=ALU.max))
                        dv(lambda e: e.tensor_scalar(out=oh2[:], in0=e2[:], scalar1=sc[:, 5:6], scalar2=None, op0=ALU.is_equal))
                        dv(lambda e: e.tensor_tensor(out=sc[:, 6:7], in0=sc[:, 5:6], in1=sc[:, 4:5], op=ALU.subtract))
                        p.op(ACT, lambda e: e.activation(out=sc[:, 7:8], in_=sc[:, 6:7], func=AF.Exp), reads=R, writes=R)
                        dv(lambda e: e.tensor_scalar(out=sc[:, 8:9], in0=sc[:, 7:8], scalar1=1.0, scalar2=None, op0=ALU.add))
                        dv(lambda e: e.reciprocal(out=sc[:, 9:10], in_=sc[:, 8:9]))
                        dv(lambda e: e.tensor_tensor(out=sc[:, 10:11], in0=sc[:, 9:10], in1=sc[:, 7:8], op=ALU.mult))
                        dv(lambda e: e.tensor_tensor(out=gates[:, ti, 0:1], in0=sc[:, 9:10], in1=sc[:, 3:4], op=ALU.mult), wr_=[GATES])
                        dv(lambda e: e.tensor_tensor(out=gates[:, ti, 1:2], in0=sc[:, 10:11], in1=sc[:, 3:4], op=ALU.mult), wr_=[GATES])
                        A13 = A1[:].rearrange("p (g j) -> p g j", g=NGRP)
                        A23 = A2[:].rearrange("p (g j) -> p g j", g=NGRP)
                        ohgb = ohg[:].unsqueeze(2).to_broadcast([128, NGRP, EPG])
                        dv(lambda e: e.tensor_tensor(out=A13, in0=ohgb, in1=oh1[:].unsqueeze(1).to_broadcast([128, NGRP, EPG]), op=ALU.mult))
                        dv(lambda e: e.tensor_tensor(out=A23, in0=ohgb, in1=oh2[:].unsqueeze(1).to_broadcast([128, NGRP, EPG]), op=ALU.mult))
                        dv(lambda e: e.tensor_tensor(out=Ab[:], in0=A1[:], in1=A2[:], op=ALU.add))
                        p.op(PE, lambda e: e.matmul(banks[6][:, 0:NEXP], usb[:], Ab[:], start=True, stop=True), reads=[USB] + R, writes=[BK[6]])
                        p.op(PE, lambda e: e.matmul(banks[7][:, 0:NEXP], onesb[:], Ab[:], start=True, stop=True), reads=[ONESB] + R, writes=[BK[7]])
                        dv(lambda e: e.tensor_tensor(out=slot[:], in0=banks[6][:, 0:NEXP], in1=cum[:], op=ALU.add), rd=[BK[6], CUM])
                        dv(lambda e: e.tensor_tensor(out=slot[:], in0=slot[:], in1=cst[:, C_EBASE:C_EBASE + NEXP], op=ALU.add), rd=[CST])
                        dv(lambda e: e.tensor_tensor(out=cum[:], in0=banks[7][:, 0:NEXP], in1=cum[:], op=ALU.add), rd=[BK[7], CUM], wr_=[CUM])
                        dv(lambda e: e.tensor_tensor(out=tmp[:], in0=A1[:], in1=slot[:], op=ALU.mult))
                        dv(lambda e: e.tensor_reduce(out=sc[:, 11:12], in_=tmp[:], axis=AX.X, op=ALU.add))
                        dv(lambda e: e.tensor_tensor(out=tmp[:], in0=A2[:], in1=slot[:], op=ALU.mult))
                        dv(lambda e: e.tensor_reduce(out=sc[:, 12:13], in_=tmp[:], axis=AX.X, op=ALU.add))
                        dv(lambda e: e.tensor_copy(out=dests[:, ti, :], in_=sc[:, 11:13]), wr_=[DESTS])
                        deferred.append(ti)
                        if dbg:
                            dv(lambda e: e.tensor_copy(out=rt[:, 0:2], in_=sc[:, 11:13]))
                            dv(lambda e: e.tensor_copy(out=rt[:, 2:4], in_=gates[:, ti, :]))
                            dv(lambda e: e.tensor_copy(out=rt[:, 4:8], in_=lg[:, 0:4]))
                            p.dma(SP, lambda q: q.dma_start(out=RT[r0:r0 + 128, :], in_=rt[:]), reads=R)
                for ti_ in deferred:
                    for k_ in range(2):
                        p.dma(POOL, lambda q: q.indirect_dma_start(out=SLOT, out_offset=bass.IndirectOffsetOnAxis(dests[:, ti_, k_:k_ + 1], 0),
                                                                   in_=tokall[:, ti_, :], in_offset=None),
                              reads=[TOKA, DESTS], writes=[SLOTD])
                p.barrier()
            if stop == 'D':
                raise _Stop()

            with ExitStack() as es:
                KB = min(8, KC)
                NKB = KC // KB
                CB2 = min(1024, D)
                NCB2 = D // CB2
                idx = [sb(es, "idx%d" % i, [128, 16], I32) for i in range(2)]
                IDX = [Buf() for _ in range(2)]
                xg = [sb(es, "xg%d" % i, [128, D], BF16) for i in range(2)]
                XG = [Buf() for _ in range(2)]
                xgT = sb(es, "xgT", [128, KC, 128], BF16)
                XGT = Buf()
                NWE = 5
                wa = [sb(es, "wa%d" % i, [128, KB * DE], BF16) for i in range(NWE)]
                WA = [Buf() for _ in range(NWE)]
                assert KB * DE >= HC * CB2
                sa = sb(es, "sa", [128, DE])
                SA = Buf()
                hid = sb(es, "hid", [128, DE], BF16)
                HID = Buf()
                hidT = sb(es, "hidT", [128, HC, 128], BF16)
                HIDT = Buf()
                ysb = [sb(es, "ysb%d" % i, [128, CB2]) for i in range(2)]
                YSB = [Buf() for _ in range(2)]
                YD = Buf()
                wcnt = [0]
                ycnt = [0]
                nhalf = max(1, DE // 512)
                hw_ = min(512, DE)

                def load_w(src3):
                    s = wcnt[0] % NWE
                    wcnt[0] += 1
                    kdim, ncol = src3.shape[1], src3.shape[2]
                    dst = wa[s][:, 0:kdim * ncol].rearrange("p (k n) -> p k n", k=kdim)
                    p.dma(POOL, lambda q: q.dma_start(out=dst, in_=src3), writes=[WA[s]])
                    return s, dst

                def prefetch(e_):
                    s = e_ % 2
                    p.dma(SP, lambda q: q.dma_start(out=idx[s][:], in_=SLOT[e_ * CAP:(e_ + 1) * CAP, :]), writes=[IDX[s]])
                    p.dma(POOL, lambda q: q.indirect_dma_start(out=xg[s][:], out_offset=None, in_=HN2,
                                                               in_offset=bass.IndirectOffsetOnAxis(idx[s][:, 0:1], 0)),
                          reads=[IDX[s]], writes=[XG[s]])
                prefetch(0)
                tbt = banks[6][:].bitcast(BF16)
                tbt2 = banks[7][:].bitcast(BF16)
                for e_ in range(NEXP):
                    s = e_ % 2
                    if e_ + 1 < NEXP:
                        prefetch(e_ + 1)
                    for k4 in range(0, KC, 4):
                        nk = min(4, KC - k4)
                        tbk = tbt if (k4 // 4) % 2 == 0 else tbt2
                        bki = 6 if (k4 // 4) % 2 == 0 else 7

                        def ftx(e):
                            r = None
                            for kc in range(k4, k4 + nk):
                                r = e.transpose(tbk[:, (kc - k4) * 128:(kc - k4 + 1) * 128], xg[s][:, kc * 128:(kc + 1) * 128], identb[:])
                            return r
                        p.op(PE, ftx, reads=[XG[s], IDB], writes=[BK[bki]])
                        eng = ACT if (k4 // 4) % 2 == 0 else DVE
                        if eng == ACT:
                            p.op(ACT, lambda e: e.activation(out=xgT[:, k4:k4 + nk, :].rearrange("p a b -> p (a b)"), in_=tbk[:, 0:nk * 128], func=AF.Copy),
                                 reads=[BK[bki]], writes=[XGT])
                        else:
                            p.op(DVE, lambda e: e.tensor_copy(out=xgT[:, k4:k4 + nk, :].rearrange("p a b -> p (a b)"), in_=tbk[:, 0:nk * 128]),
                                 reads=[BK[bki]], writes=[XGT])
                    for wi, wsrc in enumerate((w1, w3)):
                        for kb_ in range(NKB):
                            src3 = wsrc[e_, kb_ * KB * 128:(kb_ + 1) * KB * 128, :].rearrange("(k p) n -> p k n", p=128)
                            ws, wv = load_w(src3)

                            def fw(e):
                                r = None
                                for kc in range(KB):
                                    for hf in range(nhalf):
                                        r = e.matmul(banks[wi * 2 + hf][:, 0:hw_], xgT[:, kb_ * KB + kc, :], wv[:, kc, hf * hw_:(hf + 1) * hw_],
                                                     start=(kb_ == 0 and kc == 0), stop=(kb_ == NKB - 1 and kc == KB - 1))
                                return r
                            p.op(PE, fw, reads=[XGT, WA[ws]], writes=[BK[wi * 2 + hf_] for hf_ in range(nhalf)])
                    for hf in range(nhalf):
                        p.op(ACT, lambda e: e.activation(out=sa[:, hf * hw_:(hf + 1) * hw_], in_=banks[hf][:, 0:hw_], func=AF.Silu),
                             reads=[BK[hf]], writes=[SA])
                        p.op(DVE, lambda e: e.tensor_tensor(out=hid[:, hf * hw_:(hf + 1) * hw_], in0=banks[2 + hf][:, 0:hw_], in1=sa[:, hf * hw_:(hf + 1) * hw_],
                                                            op=ALU.mult), reads=[BK[2 + hf], SA], writes=[HID])
                    for k4 in range(0, HC, 4):
                        nk = min(4, HC - k4)

                        def fth(e):
                            r = None
                            for kc in range(k4, k4 + nk):
                                r = e.transpose(tbt[:, (kc - k4) * 128:(kc - k4 + 1) * 128], hid[:, kc * 128:(kc + 1) * 128], identb[:])
                            return r
                        p.op(PE, fth, reads=[HID, IDB], writes=[BK[6]])
                        p.op(ACT, lambda e: e.activation(out=hidT[:, k4:k4 + nk, :].rearrange("p a b -> p (a b)"), in_=tbt[:, 0:nk * 128], func=AF.Copy),
                             reads=[BK[6]], writes=[HIDT])
                    for cb in range(NCB2):
                        src3 = w2[e_, :, cb * CB2:(cb + 1) * CB2].rearrange("(k p) n -> p k n", p=128)
                        ws, wv = load_w(src3)
                        ys = ycnt[0] % 2
                        ycnt[0] += 1
                        for hf in range(CB2 // 512):
                            bk = 4 + hf

                            def fy(e):
                                r = None
                                for kc in range(HC):
                                    r = e.matmul(banks[bk][:], hidT[:, kc, :], wv[:, kc, hf * 512:(hf + 1) * 512], start=(kc == 0), stop=(kc == HC - 1))
                                return r
                            p.op(PE, fy, reads=[HIDT, WA[ws]], writes=[BK[bk]])
                            if hf % 2 == 0:
                                p.op(ACT, lambda e: e.activation(out=ysb[ys][:, hf * 512:(hf + 1) * 512], in_=banks[bk][:], func=AF.Copy),
                                     reads=[BK[bk]], writes=[YSB[ys]])
                            else:
                                p.op(DVE, lambda e: e.tensor_copy(out=ysb[ys][:, hf * 512:(hf + 1) * 512], in_=banks[bk][:]),
                                     reads=[BK[bk]], writes=[YSB[ys]])
                        p.dma(SP, lambda q: q.dma_start(out=Y[e_ * CAP:(e_ + 1) * CAP, cb * CB2:(cb + 1) * CB2], in_=ysb[ys][:]),
                              reads=[YSB[ys]], writes=[YD])
                p.barrier()
            if stop == 'F':
                raise _Stop()

            with ExitStack() as es:
                hx = [sb(es, "hx%d" % i, [128, D]) for i in range(2)]
                y1 = [sb(es, "y1_%d" % i, [128, D]) for i in range(2)]
                y2 = [sb(es, "y2_%d" % i, [128, D]) for i in range(2)]
                HX = [Buf() for _ in range(2)]
                Y1 = [Buf() for _ in range(2)]
                Y2 = [Buf() for _ in range(2)]
                lnft = sb(es, "lnft", [128, D])
                junk = sb(es, "junk3", [128, D], BF16)
                JUNK = Buf()
                SM = Buf()
                st = [sb(es, "gst%d" % i, [128, 1]) for i in range(3)]
                ST = [Buf() for _ in range(3)]
                p.dma(SP, lambda q: q.dma_start(out=lnft[:], in_=lnfw.partition_broadcast(128)), writes=[SM])
                for ti in range(NT):
                    s = ti % 2
                    r0 = ti * 128
                    p.dma(SP, lambda q: q.dma_start(out=hx[s][:], in_=H[r0:r0 + 128, :]), writes=[HX[s]])
                    p.dma(POOL, lambda q: q.indirect_dma_start(out=y1[s][:], out_offset=None, in_=Y,
                                                               in_offset=bass.IndirectOffsetOnAxis(dests[:, ti, 0:1], 0)),
                          reads=[DESTS], writes=[Y1[s]])
                    p.dma(POOL, lambda q: q.indirect_dma_start(out=y2[s][:], out_offset=None, in_=Y,
                                                               in_offset=bass.IndirectOffsetOnAxis(dests[:, ti, 1:2], 0)),
                          reads=[DESTS], writes=[Y2[s]])
                    p.op(DVE, lambda e: e.scalar_tensor_tensor(out=hx[s][:], in0=y1[s][:], scalar=gates[:, ti, 0:1], in1=hx[s][:],
                                                               op0=ALU.mult, op1=ALU.add), reads=[Y1[s], GATES, HX[s]], writes=[HX[s]])
                    p.op(DVE, lambda e: e.scalar_tensor_tensor(out=hx[s][:], in0=y2[s][:], scalar=gates[:, ti, 1:2], in1=hx[s][:],
                                                               op0=ALU.mult, op1=ALU.add), reads=[Y2[s], GATES, HX[s]], writes=[HX[s]])
                    p.op(ACT, lambda e: e.activation(out=junk[:], in_=hx[s][:], func=AF.Square, accum_out=st[0][:]),
                         reads=[HX[s]], writes=[JUNK, ST[0]])
                    p.op(ACT, lambda e: e.activation(out=st[1][:], in_=st[0][:], func=AF.Sqrt, bias=epsn[:], scale=1.0 / D),
                         reads=[ST[0], EPS], writes=[ST[1]])
                    p.op(DVE, lambda e: e.reciprocal(out=st[2][:], in_=st[1][:]), reads=[ST[1]], writes=[ST[2]])
                    p.op(DVE, lambda e: e.scalar_tensor_tensor(out=y1[s][:], in0=hx[s][:], scalar=st[2][:, 0:1], in1=lnft[:],
                                                               op0=ALU.mult, op1=ALU.mult), reads=[HX[s], ST[2], SM], writes=[Y1[s]])
                    p.dma(SP, lambda q: q.dma_start(out=out[r0:r0 + 128, :], in_=y1[s][:]), reads=[Y1[s]])
        except _Stop:
            pass
        print('total ops', p.nops)
        p.wait_all(SP)
    return nc


def make_in_maps(cfg, inputs):
    f = lambda a: np.ascontiguousarray(np.asarray(a))
    x = f(inputs["x"])
    positions = f(inputs["positions"]).astype(np.int32)
    T, D, KC, NCB = cfg.T, cfg.D, cfg.KC, cfg.NCB
    l = 0
    ln1w = f(inputs["ln1_w"][l]).reshape(KC, 128).T.copy()
    w_in = f(inputs["w_in"][l])
    convw = f(inputs["conv_w"][l]).reshape(3, NCB, 128).transpose(2, 1, 0).reshape(128, NCB * 3).copy()
    cnw = f(inputs["conv_norm_w"][l]).reshape(NCB, 128).T.copy()
    lam4 = np.concatenate([f(inputs["lam_q1"][l]), f(inputs["lam_k1"][l]), f(inputs["lam_q2"][l]), f(inputs["lam_k2"][l])]).reshape(1, 256)
    subw = f(inputs["subln_w"][l]).reshape(1, 128)
    w_out = f(inputs["w_out"][l])
    ln2w = f(inputs["ln2_w"][l]).reshape(1, D)
    lnfw = f(inputs["lnf_w"]).reshape(1, D)
    wr = np.concatenate([f(inputs["w_router_group"][l]), f(inputs["w_router_expert"][l])], axis=1)
    br = np.concatenate([f(inputs["b_router_group"][l]), f(inputs["b_router_expert"][l])]).reshape(1, cfg.NR)
    w1 = f(inputs["w1"][l])
    w3 = f(inputs["w3"][l])
    w2 = f(inputs["w2"][l])
    consts = make_consts(cfg)
    NTT = 2 * T // 128
    maps = []
    for c in range(cfg.NC):
        b, half = c // 2, c % 2
        xo = x[b, half * T:(half + 1) * T]
        if half == 1:
            xp = x[b, 0:T]
            ppos = positions[b, 0:T]
        else:
            xp = np.zeros_like(xo)
            ppos = np.zeros(T, np.int32)
        pos = np.concatenate([ppos, positions[b, half * T:(half + 1) * T]]).reshape(1, 2 * T).astype(np.int32)
        kb = np.zeros((128, NTT), np.float32)
        if half == 0:
            kb[:, 0:NTT // 2] = NEG
        maps.append(dict(xo=xo, xp=xp, pos=pos, kb=kb, ln1w=ln1w, w_in=w_in, convw=convw, cnw=cnw, lam4=lam4, subw=subw,
                         w_out=w_out, ln2w=ln2w, lnfw=lnfw, wr=wr, br=br, w1=w1, w3=w3, w2=w2, consts=consts))
    return maps


def kernel(**inputs):
    cfg = Cfg()
    lam_init = 0.8 - 0.6 * math.exp(-0.3 * 0)
    nc = build_nc(cfg, lam_init=lam_init)
    maps = make_in_maps(cfg, inputs)
    res = run_bass_kernel_spmd(nc, maps, core_ids=list(range(cfg.NC)))
    outp = np.zeros((cfg.B, cfg.S, cfg.D), np.float32)
    for c in range(cfg.NC):
        b, half = c // 2, c % 2
        outp[b, half * cfg.T:(half + 1) * cfg.T] = res.results[c]["out"]
    return outp
```

```python
import math
from contextlib import ExitStack
import numpy as np
import concourse.bass as bass
import concourse.mybir as mybir
from concourse.bass_utils import run_bass_kernel_spmd

F32 = mybir.dt.float32
BF16 = mybir.dt.bfloat16
I32 = mybir.dt.int32
ALU = mybir.AluOpType
AF = mybir.ActivationFunctionType
AX = mybir.AxisListType

PE, ACT, DVE, POOL, SP = 0, 1, 2, 3, 4


class Ev:
    __slots__ = ("kind", "eng", "seq", "sem", "semid", "val", "clock", "dclock")


class Buf:
    __slots__ = ("name", "w", "r", "excl")

    def __init__(self, name="", excl=False):
        self.name = name
        self.w = None
        self.r = []
        self.excl = excl


class Eng:
    def __init__(self, idx, b, sem):
        self.idx = idx
        self.b = b
        self.sem = sem
        self.count = 0
        self.known = [0] * 5
        self.kd = {}


class Prog:
    def __init__(self, nc, es, n_dma_sems=16):
        self.nc = nc
        builders = [nc.tensor, nc.scalar, nc.vector, nc.gpsimd, nc.sync]
        self.E = []
        for i, b in enumerate(builders):
            sem = es.enter_context(nc.semaphore("esem%d" % i))
            self.E.append(Eng(i, b, sem))
        self.dsems = {}
        for q in (SP, POOL, ACT):
            ring = []
            for j in range(n_dma_sems):
                sem = es.enter_context(nc.semaphore("dsem%d_%d" % (q, j)))
                ring.append([sem, 0, None])
            self.dsems[q] = [ring, 0]

    def _merge(self, E, ev):
        k = E.known
        c = ev.clock
        for i in range(5):
            if c[i] > k[i]:
                k[i] = c[i]
        kd = E.kd
        for s, v in ev.dclock.items():
            if kd.get(s, 0) < v:
                kd[s] = v

    def _wait(self, E, ev):
        if ev.kind == 0:
            if ev.eng is E and E.idx == PE:
                return
            if E.known[ev.eng.idx] >= ev.seq:
                return
            E.b.wait_ge(ev.eng.sem, ev.seq)
            E.known[ev.eng.idx] = ev.seq
            self._merge(E, ev)
        else:
            if E.kd.get(ev.semid, 0) >= ev.val:
                return
            E.b.wait_ge(ev.sem, ev.val)
            E.kd[ev.semid] = ev.val
            self._merge(E, ev)

    def _deps(self, E, reads, writes):
        for b in reads:
            if b.w is not None:
                self._wait(E, b.w)
        for b in writes:
            if b.w is not None:
                self._wait(E, b.w)
            for r in b.r:
                self._wait(E, r)

    def _post(self, ev, reads, writes):
        for b in reads:
            b.r.append(ev)
        for b in writes:
            b.w = ev
            b.r = []

    limit = None
    nops = 0

    def op(self, e, fn, reads=(), writes=()):
        self.nops += 1
        if self.limit is not None and self.nops > self.limit:
            return None
        E = self.E[e]
        writes = list(writes) + [b for b in reads if b.excl]
        reads = [b for b in reads if not b.excl]
        self._deps(E, reads, writes)
        inst = fn(E.b)
        E.count += 1
        inst.then_inc(E.sem, 1)
        ev = Ev()
        ev.kind = 0
        ev.eng = E
        ev.seq = E.count
        ev.clock = list(E.known)
        ev.dclock = dict(E.kd)
        self._post(ev, reads, writes)
        return ev

    def dma(self, q, fn, reads=(), writes=()):
        self.nops += 1
        if self.limit is not None and self.nops > self.limit:
            return None
        E = self.E[q]
        self._deps(E, reads, writes)
        ringinfo = self.dsems[q]
        ring, pos = ringinfo
        slot = ring[pos % len(ring)]
        ringinfo[1] = pos + 1
        if slot[2] is not None:
            self._wait(E, slot[2])
        inst = fn(E.b)
        slot[1] += 1
        inst.then_inc(slot[0], 16)
        ev = Ev()
        ev.kind = 1
        ev.sem = slot[0]
        ev.semid = (q, pos % len(ring))
        ev.val = 16 * slot[1]
        ev.clock = list(E.known)
        ev.dclock = dict(E.kd)
        slot[2] = ev
        self._post(ev, reads, writes)
        return ev

    def _all_events(self):
        evs = []
        for E in self.E:
            if E.count > 0:
                ev = Ev()
                ev.kind = 0
                ev.eng = E
                ev.seq = E.count
                ev.clock = [0] * 5
                ev.dclock = {}
                evs.append(ev)
        for q, (ring, pos) in self.dsems.items():
            for slot in ring:
                if slot[2] is not None:
                    evs.append(slot[2])
        return evs

    def barrier(self):
        evs = self._all_events()
        for E in self.E:
            for ev in evs:
                self._wait(E, ev)

    def wait_all(self, e):
        E = self.E[e]
        for ev in self._all_events():
            self._wait(E, ev)


class _Stop(Exception):
    pass


class Cfg:
    def __init__(self, D=4096, S=4096, B=4, NEXP=64, NGRP=8):
        self.D, self.S, self.B = D, S, B
        self.T = S // 2
        self.NC = 2 * B
        self.KC = D // 128
        self.CW = D // 2
        self.AW = D // 2
        self.NCB = self.CW // 128
        self.NH = self.AW // 128
        self.INC = 3 * self.CW + 3 * self.AW
        self.DE = D // 4
        self.HC = self.DE // 128
        self.NEXP = NEXP
        self.NGRP = NGRP
        self.EPG = NEXP // NGRP
        self.NT = self.T // 128
        self.NG = self.T // 512
        self.CAP = 128
        self.NR = NGRP + NEXP


MAGIC = 12582912.0
TWO_PI = 2.0 * math.pi
CW1 = 6.28125
CW2 = float(np.float32(TWO_PI - CW1))
NEG = -30000.0

C_ID, C_RP, C_TRI, C_US, C_ONES = 0, 128, 256, 384, 512
C_INVF, C_PIDX, C_EBASE = 640, 641, 642


def make_consts(cfg):
    n = C_EBASE + cfg.NEXP
    c = np.zeros((128, n), np.float32)
    c[:, C_ID:C_ID + 128] = np.eye(128, dtype=np.float32)
    rp = np.zeros((128, 128), np.float32)
    for m in range(128):
        if m % 64 < 32:
            rp[m + 32, m] = -1.0
        else:
            rp[m - 32, m] = 1.0
    c[:, C_RP:C_RP + 128] = rp
    k = np.arange(128)[:, None]
    q = np.arange(128)[None, :]
    c[:, C_TRI:C_TRI + 128] = (q >= k).astype(np.float32)
    c[:, C_US:C_US + 128] = (k < q).astype(np.float32)
    c[:, C_ONES:C_ONES + 128] = 1.0
    half = 32
    inv = (1.0 / (10000.0 ** (np.arange(half, dtype=np.float32) * 2.0 / 64.0))).astype(np.float32)
    c[:, C_INVF] = inv[np.arange(128) % 32]
    c[:, C_PIDX] = np.arange(128, dtype=np.float32)
    c[:, C_EBASE:C_EBASE + cfg.NEXP] = (np.arange(cfg.NEXP, dtype=np.float32) * cfg.CAP)[None, :]
    return c


def build_nc(cfg, lam_init=0.2, dbg=False, stop=None):
    D, T, KC, NCB, NH, DE, HC, NEXP, NT, NG = (cfg.D, cfg.T, cfg.KC, cfg.NCB, cfg.NH, cfg.DE,
                                              cfg.HC, cfg.NEXP, cfg.NT, cfg.NG)
    CW, AW, INC, NR, CAP, NGRP, EPG = cfg.CW, cfg.AW, cfg.INC, cfg.NR, cfg.CAP, cfg.NGRP, cfg.EPG
    TT = 2 * T
    NTT = TT // 128
    NCONST = C_EBASE + NEXP
    nc = bass.Bass("TRN2", target_bir_lowering=False)

    def din(name, shape, dt=F32):
        return nc.dram_tensor(name, list(shape), dt, kind="ExternalInput").ap()

    def dscr(name, shape, dt):
        return nc.dram_tensor(name, list(shape), dt, kind=("ExternalOutput" if dbg else "Internal")).ap()

    xo = din("xo", [T, D])
    xp = din("xp", [T, D])
    pos = din("pos", [1, TT], I32)
    kb = din("kb", [128, NTT])
    ln1w = din("ln1w", [128, KC])
    w_in = din("w_in", [D, INC])
    convw = din("convw", [128, NCB * 3])
    cnw = din("cnw", [128, NCB])
    lam4 = din("lam4", [1, 256])
    subw = din("subw", [1, 128])
    w_out = din("w_out", [D, D])
    ln2w = din("ln2w", [1, D])
    lnfw = din("lnfw", [1, D])
    wr = din("wr", [D, NR])
    br = din("br", [1, NR])
    w1 = din("w1", [NEXP, D, DE])
    w3 = din("w3", [NEXP, D, DE])
    w2 = din("w2", [NEXP, DE, D])
    consts = din("consts", [128, NCONST])
    out = nc.dram_tensor("out", [T, D], F32, kind="ExternalOutput").ap()

    QT = dscr("QT", [NH, 128, T], BF16)
    KT = dscr("KT", [NH, 128, TT], BF16)
    VS = dscr("VS", [NH, 128, NTT, 128], BF16)
    MIXT = dscr("MIXT", [D, T], BF16)
    H = dscr("H", [T, D], F32)
    HN2 = dscr("HN2", [T + 128, D], BF16)
    Y = dscr("Y", [NEXP * CAP, D], F32)
    SLOT = dscr("SLOT", [NEXP * CAP, 16], I32)
    NPRE = 0
    W1B = W3B = W2B = None
    if NPRE > 0:
        W1B = nc.dram_tensor("W1B", [NPRE, D, DE], BF16, kind="Internal").ap()
        W3B = nc.dram_tensor("W3B", [NPRE, D, DE], BF16, kind="Internal").ap()
        W2B = nc.dram_tensor("W2B", [NPRE, DE, D], BF16, kind="Internal").ap()
    RT = dscr("RT", [T, 8], F32)

    with ExitStack() as ges:
        p = Prog(nc, ges)
        import os
        if os.environ.get('OPLIMIT'):
            p.limit = int(os.environ['OPLIMIT'])

        def sb(es, name, shape, dt=F32):
            return es.enter_context(nc.sbuf_tensor(name, list(shape), dt))

        cst = sb(ges, "cst", [128, NCONST])
        identb = sb(ges, "identb", [128, 128], BF16)
        rpermf = cst[:, C_RP:C_RP + 128]
        trib = sb(ges, "trib", [128, 128], BF16)
        usb = sb(ges, "usb", [128, 128], BF16)
        onesb = sb(ges, "onesb", [128, 128], BF16)
        meanb = sb(ges, "meanb", [128, 128], BF16)
        epsn = sb(ges, "epsn", [128, 1])
        epss = sb(ges, "epss", [128, 1])
        nlam = sb(ges, "nlam", [128, 1])
        dests = sb(ges, "dests", [128, NT, 2], I32)
        gates = sb(ges, "gates", [128, NT, 2])
        CST, IDB, TRIB, USB, ONESB, MEANB, EPS, NLAM, DESTS, GATES = [Buf() for _ in range(10)]
        banks = [ges.enter_context(nc.psum_tensor("bank%d" % i, [128, 512], F32)) for i in range(8)]
        BK = [Buf("bank%d" % i, excl=True) for i in range(8)]

        p.dma(SP, lambda q: q.dma_start(out=cst[:], in_=consts), writes=[CST])
        p.op(DVE, lambda e: e.tensor_copy(out=identb[:], in_=cst[:, C_ID:C_ID + 128]), reads=[CST], writes=[IDB])
        p.op(DVE, lambda e: e.tensor_copy(out=trib[:], in_=cst[:, C_TRI:C_TRI + 128]), reads=[CST], writes=[TRIB])
        p.op(DVE, lambda e: e.tensor_copy(out=usb[:], in_=cst[:, C_US:C_US + 128]), reads=[CST], writes=[USB])
        p.op(DVE, lambda e: e.tensor_copy(out=onesb[:], in_=cst[:, C_ONES:C_ONES + 128]), reads=[CST], writes=[ONESB])
        p.op(DVE, lambda e: e.tensor_scalar(out=meanb[:], in0=cst[:, C_ONES:C_ONES + 128], scalar1=1.0 / 128.0,
                                            scalar2=None, op0=ALU.mult), reads=[CST], writes=[MEANB])
        p.op(DVE, lambda e: e.memset(epsn[:], 1e-6), writes=[EPS])
        p.op(DVE, lambda e: e.memset(epss[:], 1e-5), writes=[EPS])

        try:
            with ExitStack() as es:
                xt = [sb(es, "xt%d" % i, [128, D]) for i in range(2)]
                XT = [Buf() for _ in range(2)]
                hnb = sb(es, "hnb", [128, D], BF16)
                HNB = Buf()
                hnT = [sb(es, "hnT%d" % i, [128, KC, 512], BF16) for i in range(2)]
                HNT = [Buf() for _ in range(2)]
                halo_hn = sb(es, "halo_hn", [128, KC, 2], BF16)
                HALOHN = Buf()
                NWR = 3
                wring = [sb(es, "wr%d" % i, [128, KC, 256], BF16) for i in range(NWR)]
                WR = [Buf() for _ in range(NWR)]
                ln1t = sb(es, "ln1t", [128, KC])
                cwt = sb(es, "cwt", [128, NCB * 3])
                cnt = sb(es, "cnt", [128, NCB])
                kbt = sb(es, "kbt", [128, NTT])
                SMALL = Buf()
                st = [sb(es, "st%d" % i, [128, 1]) for i in range(3)]
                ST = [Buf() for _ in range(3)]
                posi = sb(es, "posi", [128, 512], I32)
                ang = sb(es, "ang", [128, 512])
                kk = sb(es, "kk", [128, 512])
                cosT = sb(es, "cosT", [128, 512])
                sinT = sb(es, "sinT", [128, 512])
                POSI, ANG, KKB, COS, SIN = [Buf() for _ in range(5)]
                zhalo = sb(es, "zhalo", [128, NCB, 2])
                ZH = Buf()
                sd = sb(es, "sd", [128, 512])
                SD = Buf()
                cout = [sb(es, "cout%d" % i, [128, 512], BF16) for i in range(2)]
                COUT = [Buf() for _ in range(2)]
                t1 = sb(es, "t1", [128, 512])
                T1 = Buf()
                t2 = sb(es, "t2", [128, 512])
                T2 = Buf()
                qk = [sb(es, "qk%d" % i, [128, 512], BF16) for i in range(2)]
                QK = [Buf() for _ in range(2)]
                vtok = [sb(es, "vtok%d" % i, [128, 4, 128], BF16) for i in range(2)]
                VTOK = [Buf() for _ in range(2)]
                lamt = sb(es, "lamt", [128, 256])
                lamp = sb(es, "lamp", [128, 128])
                LAM = Buf()

                p.dma(SP, lambda q: q.dma_start(out=ln1t[:], in_=ln1w), writes=[SMALL])
                p.dma(SP, lambda q: q.dma_start(out=cwt[:], in_=convw), writes=[SMALL])
                p.dma(SP, lambda q: q.dma_start(out=cnt[:], in_=cnw), writes=[SMALL])
                p.dma(SP, lambda q: q.dma_start(out=kbt[:], in_=kb), writes=[SMALL])
                p.op(DVE, lambda e: e.memset(zhalo[:], 0.0), writes=[ZH])
                p.op(DVE, lambda e: e.memset(halo_hn[:], 0.0), writes=[HALOHN])
                p.dma(SP, lambda q: q.dma_start(out=lamt[:], in_=lam4.partition_broadcast(128)), writes=[LAM])
                p.op(DVE, lambda e: e.tensor_tensor(out=lamp[:, 0:64], in0=lamt[:, 0:64], in1=lamt[:, 64:128], op=ALU.mult), reads=[LAM], writes=[LAM])
                p.op(DVE, lambda e: e.tensor_tensor(out=lamp[:, 64:128], in0=lamt[:, 128:192], in1=lamt[:, 192:256], op=ALU.mult), reads=[LAM], writes=[LAM])
                p.op(DVE, lambda e: e.tensor_reduce(out=st[0][:], in_=lamp[:, 0:64], axis=AX.X, op=ALU.add), reads=[LAM], writes=[ST[0]])
                p.op(DVE, lambda e: e.tensor_reduce(out=st[1][:], in_=lamp[:, 64:128], axis=AX.X, op=ALU.add), reads=[LAM], writes=[ST[1]])
                p.op(ACT, lambda e: e.activation(out=st[0][:], in_=st[0][:], func=AF.Exp), reads=[ST[0]], writes=[ST[0]])
                p.op(ACT, lambda e: e.activation(out=st[1][:], in_=st[1][:], func=AF.Exp), reads=[ST[1]], writes=[ST[1]])
                p.op(DVE, lambda e: e.tensor_tensor(out=st[2][:], in0=st[1][:], in1=st[0][:], op=ALU.subtract), reads=[ST[0], ST[1]], writes=[ST[2]])
                p.op(DVE, lambda e: e.tensor_scalar(out=nlam[:], in0=st[2][:], scalar1=-float(lam_init), scalar2=None, op0=ALU.add),
                     reads=[ST[2]], writes=[NLAM])

                yb2 = [sb(es, "yb2_%d" % i, [128, 512]) for i in range(2)]
                ysq2 = [sb(es, "ysq2_%d" % i, [128, 512], BF16) for i in range(2)]
                qf2 = [sb(es, "qf2_%d" % i, [128, 512]) for i in range(2)]
                vb2 = [sb(es, "vb2_%d" % i, [128, 512], BF16) for i in range(2)]
                YB2 = [Buf() for _ in range(2)]
                YSQ2 = [Buf() for _ in range(2)]
                QF2 = [Buf() for _ in range(2)]
                VB2 = [Buf() for _ in range(2)]
                cs2 = [sb(es, "cs2_%d" % i, [128, 512]) for i in range(2)]
                zt2 = [sb(es, "zt2_%d" % i, [128, 514]) for i in range(2)]
                acc2 = [sb(es, "acc2_%d" % i, [128, 512]) for i in range(2)]
                CS2 = [Buf() for _ in range(2)]
                ZT2 = [Buf() for _ in range(2)]
                ACC2 = [Buf() for _ in range(2)]
                ucount = [0]
                wcount = [0]
                pcount = [0]
                xcount = [0]

                def load_wblk(col0):
                    s = wcount[0] % NWR
                    wcount[0] += 1
                    src = w_in[:, col0:col0 + 256].rearrange("(k p) n -> p k n", p=128)
                    p.dma(POOL, lambda q: q.dma_start(out=wring[s][:], in_=src), writes=[WR[s]])
                    return s

                def proj(s, sub=0, ncols=512, col_lo=0, rhs_t=None, bank=None):
                    if bank is None:
                        bank = pcount[0] % 4
                        pcount[0] += 1
                    src = hnT[curg[0]] if rhs_t is None else rhs_t

                    def f(e):
                        r = None
                        for kc in range(KC):
                            r = e.matmul(banks[bank][:, 0:ncols], wring[s][:, kc, sub * 128:(sub + 1) * 128], src[:, kc, col_lo:col_lo + ncols],
                                         start=(kc == 0), stop=(kc == KC - 1))
                        return r
                    p.op(PE, f, reads=[WR[s], HNT[curg[0]] if rhs_t is None else HALOHN], writes=[BK[bank]])
                    return bank

                def issue_x(n_):
                    gi_, i_ = n_ // 4, n_ % 4
                    src_ = xp if gi_ < NG else xo
                    g_ = gi_ if gi_ < NG else gi_ - NG
                    r0_ = g_ * 512 + i_ * 128
                    p.dma(SP, lambda q: q.dma_start(out=xt[n_ % 2][:], in_=src_[r0_:r0_ + 128, :]), writes=[XT[n_ % 2]])

                def prep_tile(gi, i):
                    is_prev = gi < NG
                    g = gi if is_prev else gi - NG
                    xsrc = xp if is_prev else xo
                    hT = hnT[gi % 2]
                    HT_ = HNT[gi % 2]
                    n_ = gi * 4 + i
                    xs = n_ % 2
                    if n_ == 0:
                        issue_x(0)
                    if n_ + 1 < 8 * NG:
                        issue_x(n_ + 1)
                    p.op(ACT, lambda e: e.activation(out=hnb[:], in_=xt[xs][:], func=AF.Square, accum_out=st[0][:]),
                         reads=[XT[xs]], writes=[HNB, ST[0]])
                    p.op(ACT, lambda e: e.activation(out=st[1][:], in_=st[0][:], func=AF.Sqrt, bias=epsn[:], scale=1.0 / D),
                         reads=[ST[0], EPS], writes=[ST[1]])
                    p.op(DVE, lambda e: e.reciprocal(out=st[2][:], in_=st[1][:]), reads=[ST[1]], writes=[ST[2]])
                    p.op(DVE, lambda e: e.tensor_scalar(out=hnb[:], in0=xt[xs][:], scalar1=st[2][:, 0:1], scalar2=None, op0=ALU.mult),
                         reads=[XT[xs], ST[2]], writes=[HNB])
                    for k4 in range(0, KC, 4):
                        bkid = 4 if (k4 // 4) % 2 == 0 else 6
                        tb = banks[bkid][:].bitcast(BF16)

                        def ftr0(e):
                            r = None
                            for kc in range(k4, k4 + 4):
                                r = e.transpose(tb[:, (kc - k4) * 128:(kc - k4 + 1) * 128], hnb[:, kc * 128:(kc + 1) * 128], identb[:])
                            return r
                        p.op(PE, ftr0, reads=[HNB, IDB], writes=[BK[bkid]])
                        for kc in range(k4, k4 + 4):
                            off = (kc - k4) * 128
                            if bkid == 6:
                                p.op(ACT, lambda e: e.activation(out=hT[:, kc, i * 128:(i + 1) * 128], in_=tb[:, off:off + 128],
                                                                 func=AF.Copy, scale=ln1t[:, kc:kc + 1]),
                                     reads=[BK[bkid], SMALL], writes=[HT_])
                            else:
                                p.op(DVE, lambda e: e.tensor_scalar(out=hT[:, kc, i * 128:(i + 1) * 128], in0=tb[:, off:off + 128],
                                                                    scalar1=ln1t[:, kc:kc + 1], scalar2=None, op0=ALU.mult),
                                     reads=[BK[bkid], SMALL], writes=[HT_])
                    if gi == NG - 1 and i == 3:
                        p.op(DVE, lambda e: e.tensor_copy(out=halo_hn[:], in_=hT[:, :, 510:512]), reads=[HT_], writes=[HALOHN])

                pending = []
                units = [0]

                def tick():
                    units[0] += 1
                    while pending and units[0] >= pending[0][0]:
                        _, a = pending.pop(0)
                        prep_tile(*a)

                for i_ in range(4):
                    prep_tile(0, i_)
                curg = [0]
                for gi in range(2 * NG):
                    is_prev = gi < NG
                    g = gi if is_prev else gi - NG
                    tok0 = gi * 512
                    curg[0] = gi % 2
                    n_units = (2 * NH) if is_prev else (NCB + 3 * NH)
                    units[0] = 0
                    if gi + 1 < 2 * NG:
                        pending[:] = [(max(1, (n_units * (k_ + 1)) // 5), (gi + 1, k_)) for k_ in range(4)]
                    p.dma(SP, lambda q: q.dma_start(out=posi[:], in_=pos[0:1, tok0:tok0 + 512].partition_broadcast(128)), writes=[POSI])
                    p.op(DVE, lambda e: e.tensor_copy(out=ang[:], in_=posi[:]), reads=[POSI], writes=[ANG])
                    p.op(DVE, lambda e: e.tensor_scalar(out=ang[:], in0=ang[:], scalar1=cst[:, C_INVF:C_INVF + 1], scalar2=None, op0=ALU.mult),
                         reads=[ANG, CST], writes=[ANG])
                    p.op(DVE, lambda e: e.tensor_scalar(out=kk[:], in0=ang[:], scalar1=1.0 / TWO_PI, scalar2=MAGIC, op0=ALU.mult, op1=ALU.add),
                         reads=[ANG], writes=[KKB])
                    p.op(DVE, lambda e: e.tensor_scalar(out=kk[:], in0=kk[:], scalar1=MAGIC, scalar2=None, op0=ALU.subtract),
                         reads=[KKB], writes=[KKB])
                    p.op(DVE, lambda e: e.scalar_tensor_tensor(out=ang[:], in0=kk[:], scalar=-CW1, in1=ang[:], op0=ALU.mult, op1=ALU.add),
                         reads=[KKB, ANG], writes=[ANG])
                    p.op(DVE, lambda e: e.scalar_tensor_tensor(out=ang[:], in0=kk[:], scalar=-CW2, in1=ang[:], op0=ALU.mult, op1=ALU.add),
                         reads=[KKB, ANG], writes=[ANG])
                    p.op(ACT, lambda e: e.activation(out=sinT[:], in_=ang[:], func=AF.Sin), reads=[ANG], writes=[SIN])
                    p.op(DVE, lambda e: e.tensor_scalar(out=ang[:], in0=ang[:], scalar1=math.pi / 2, scalar2=None, op0=ALU.add),
                         reads=[ANG], writes=[ANG])
                    p.op(DVE, lambda e: e.tensor_scalar(out=kk[:], in0=ang[:], scalar1=math.pi, scalar2=-TWO_PI, op0=ALU.is_gt, op1=ALU.mult),
                         reads=[ANG], writes=[KKB])
                    p.op(DVE, lambda e: e.tensor_tensor(out=ang[:], in0=ang[:], in1=kk[:], op=ALU.add), reads=[ANG, KKB], writes=[ANG])
                    p.op(ACT, lambda e: e.activation(out=cosT[:], in_=ang[:], func=AF.Sin), reads=[ANG], writes=[COS])

                    deferred_pe = []

                    def flush_deferred():
                        while deferred_pe:
                            deferred_pe.pop(0)()

                    if not is_prev:
                        for j0 in range(0, NCB, 2):
                            sC = load_wblk(CW + j0 * 128)
                            sU = load_wblk(2 * CW + j0 * 128)
                            sB = load_wblk(j0 * 128)
                            bCs = []
                            for sub in range(2):
                                bC = proj(sC, sub)
                                flush_deferred()
                                p.op(ACT, lambda e: e.activation(out=cs2[sub][:], in_=banks[bC][:], func=AF.Copy), reads=[BK[bC]], writes=[CS2[sub]])
                            for sub in range(2):
                                j = j0 + sub
                                zt_ = zt2[sub]
                                ZT_ = ZT2[sub]
                                acc_ = acc2[sub]
                                ACC_ = ACC2[sub]
                                bU = proj(sU, sub)
                                if g == 0:
                                    proj(sC, sub, ncols=2, rhs_t=halo_hn, bank=6)
                                    p.op(ACT, lambda e: e.activation(out=zt_[:, 0:2], in_=banks[6][:, 0:2], func=AF.Copy), reads=[BK[6]], writes=[ZT_])
                                    proj(sU, sub, ncols=2, rhs_t=halo_hn, bank=6)
                                    p.op(DVE, lambda e: e.tensor_tensor(out=zt_[:, 0:2], in0=banks[6][:, 0:2], in1=zt_[:, 0:2], op=ALU.mult),
                                         reads=[BK[6], ZT_], writes=[ZT_])
                                else:
                                    p.op(DVE, lambda e: e.tensor_copy(out=zt_[:, 0:2], in_=zhalo[:, j, :]), reads=[ZH], writes=[ZT_])
                                p.op(DVE, lambda e: e.tensor_tensor(out=zt_[:, 2:514], in0=banks[bU][:], in1=cs2[sub][:], op=ALU.mult),
                                     reads=[BK[bU], CS2[sub]], writes=[ZT_])
                                p.op(DVE, lambda e: e.tensor_copy(out=zhalo[:, j, :], in_=zt_[:, 512:514]), reads=[ZT_], writes=[ZH])
                                p.op(DVE, lambda e: e.tensor_scalar(out=acc_[:], in0=zt_[:, 2:514], scalar1=cwt[:, j * 3 + 2:j * 3 + 3], scalar2=None, op0=ALU.mult),
                                     reads=[ZT_, SMALL], writes=[ACC_])
                                p.op(DVE, lambda e: e.scalar_tensor_tensor(out=acc_[:], in0=zt_[:, 1:513], scalar=cwt[:, j * 3 + 1:j * 3 + 2], in1=acc_[:],
                                                                            op0=ALU.mult, op1=ALU.add), reads=[ZT_, SMALL, ACC_], writes=[ACC_])
                                p.op(DVE, lambda e: e.scalar_tensor_tensor(out=acc_[:], in0=zt_[:, 0:512], scalar=cwt[:, j * 3:j * 3 + 1], in1=acc_[:],
                                                                            op0=ALU.mult, op1=ALU.add), reads=[ZT_, SMALL, ACC_], writes=[ACC_])
                            for sub in range(2):
                                j = j0 + sub
                                bB = proj(sB, sub)
                                pb = ucount[0] % 2
                                ucount[0] += 1
                                p.op(DVE, lambda e: e.tensor_tensor(out=yb2[pb][:], in0=banks[bB][:], in1=acc2[sub][:], op=ALU.mult),
                                     reads=[BK[bB], ACC2[sub]], writes=[YB2[pb]])
                                p.op(ACT, lambda e: e.activation(out=ysq2[pb][:], in_=yb2[pb][:], func=AF.Square), reads=[YB2[pb]], writes=[YSQ2[pb]])

                                def rest_conv(j=j, pb=pb, g=g):
                                    p.op(PE, lambda e: e.matmul(banks[6][:], meanb[:], ysq2[pb][:], start=True, stop=True), reads=[MEANB, YSQ2[pb]], writes=[BK[6]])
                                    p.op(ACT, lambda e: e.activation(out=sd[:], in_=banks[6][:], func=AF.Sqrt, bias=epsn[:], scale=1.0),
                                         reads=[BK[6], EPS], writes=[SD])
                                    p.op(DVE, lambda e: e.reciprocal(out=sd[:], in_=sd[:]), reads=[SD], writes=[SD])
                                    co = j % 2
                                    p.op(DVE, lambda e: e.scalar_tensor_tensor(out=cout[co][:], in0=yb2[pb][:], scalar=cnt[:, j:j + 1], in1=sd[:],
                                                                               op0=ALU.mult, op1=ALU.mult), reads=[YB2[pb], SMALL, SD], writes=[COUT[co]])
                                    p.dma(SP, lambda q: q.dma_start(out=MIXT[j * 128:(j + 1) * 128, g * 512:(g + 1) * 512], in_=cout[co][:]),
                                          reads=[COUT[co]])
                                flush_deferred()
                                deferred_pe.append(rest_conv)
                                tick()
                    for h0, which, sub in [(h0_, w_, sub_) for h0_ in range(0, NH, 2) for w_ in ((1,) if is_prev else (0, 1)) for sub_ in range(2)]:
                        if True:
                            h = h0 + sub
                            if sub == 0:
                                s_qk = load_wblk(3 * CW + which * AW + h0 * 128)
                            s = s_qk
                            bq = proj(s, sub)
                            flush_deferred()
                            pb = ucount[0] % 2
                            ucount[0] += 1
                            p.op(ACT, lambda e: e.activation(out=qf2[pb][:], in_=banks[bq][:], func=AF.Copy), reads=[BK[bq]], writes=[QF2[pb]])

                            def rest_qk(h=h, which=which, pb=pb, g=g, tok0=tok0):
                                p.op(PE, lambda e: e.matmul(banks[5][:], rpermf, qf2[pb][:], start=True, stop=True), reads=[CST, QF2[pb]], writes=[BK[5]])
                                p.op(DVE, lambda e: e.tensor_tensor(out=t1[:], in0=qf2[pb][:], in1=cosT[:], op=ALU.mult), reads=[QF2[pb], COS], writes=[T1])
                                p.op(DVE, lambda e: e.tensor_tensor(out=t2[:], in0=banks[5][:], in1=sinT[:], op=ALU.mult), reads=[BK[5], SIN], writes=[T2])
                                o = (h * 2 + which) % 2
                                p.op(DVE, lambda e: e.tensor_tensor(out=qk[o][:], in0=t1[:], in1=t2[:], op=ALU.add), reads=[T1, T2], writes=[QK[o]])
                                if which == 0:
                                    p.dma(SP, lambda q: q.dma_start(out=QT[h, :, g * 512:(g + 1) * 512], in_=qk[o][:]), reads=[QK[o]])
                                else:
                                    p.dma(SP, lambda q: q.dma_start(out=KT[h, :, tok0:tok0 + 512], in_=qk[o][:]), reads=[QK[o]])
                            deferred_pe.append(rest_qk)
                            tick()
                    for h in range(NH):
                        if h % 2 == 0:
                            s_v = load_wblk(3 * CW + 2 * AW + h * 128)
                        s = s_v
                        bv = proj(s, h % 2)
                        flush_deferred()
                        pb = ucount[0] % 2
                        ucount[0] += 1
                        p.op(ACT, lambda e: e.activation(out=vb2[pb][:], in_=banks[bv][:], func=AF.Copy), reads=[BK[bv]], writes=[VB2[pb]])

                        def rest_v(h=h, pb=pb, tok0=tok0):
                            tb7 = banks[7][:].bitcast(BF16)

                            def ftr(e):
                                r = None
                                for i in range(4):
                                    r = e.transpose(tb7[:, i * 128:(i + 1) * 128], vb2[pb][:, i * 128:(i + 1) * 128], identb[:])
                                return r
                            p.op(PE, ftr, reads=[VB2[pb], IDB], writes=[BK[7]])
                            vo = h % 2
                            p.op(DVE, lambda e: e.tensor_copy(out=vtok[vo][:].rearrange("p a b -> p (a b)"), in_=tb7[:, 0:512]),
                                 reads=[BK[7]], writes=[VTOK[vo]])
                            t0 = tok0 // 128
                            p.dma(SP, lambda q: q.dma_start(out=VS[h, :, t0:t0 + 4, :], in_=vtok[vo][:]), reads=[VTOK[vo]])
                        deferred_pe.append(rest_v)
                        tick()
                    flush_deferred()
                    while pending:
                        _, a_ = pending.pop(0)
                        prep_tile(*a_)
                p.barrier()
            if stop == 'A':
                raise _Stop()

            with ExitStack() as es:
                qT = [sb(es, "qT%d" % i, [128, T], BF16) for i in range(2)]
                kT = [[sb(es, "kT%d_%d" % (i, c_), [128, TT], BF16) for c_ in range(2)] for i in range(2)]
                vS = [sb(es, "vS%d" % i, [128, NTT, 130], BF16) for i in range(2)]
                HB = [Buf() for _ in range(2)]
                NER = 8
                et = [sb(es, "et%d" % i, [128, 512], BF16) for i in range(NER)]
                ET = [Buf() for _ in range(NER)]
                kbt = sb(es, "kbt2", [128, NTT])
                subt = sb(es, "subt", [128, 128])
                SM = Buf()
                rz = sb(es, "rz", [128, 4])
                RZ = Buf()
                o1 = sb(es, "o1", [128, 128])
                O1 = Buf()
                o2 = sb(es, "o2", [128, 128])
                O2 = Buf()
                osq = sb(es, "osq", [128, 128])
                OSQ = Buf()
                of_ = sb(es, "of", [128, 128], BF16)
                OF = Buf()
                ao = [sb(es, "ao%d" % i, [128, 512], BF16) for i in range(2)]
                AO = [Buf() for _ in range(2)]
                p.dma(SP, lambda q: q.dma_start(out=kbt[:], in_=kb), writes=[SM])
                p.dma(SP, lambda q: q.dma_start(out=subt[:], in_=subw.partition_broadcast(128)), writes=[SM])
                p.op(DVE, lambda e: e.tensor_scalar(out=subt[:], in0=subt[:], scalar1=float(1.0 - lam_init), scalar2=None, op0=ALU.mult),
                     reads=[SM], writes=[SM])
                for i in range(2):
                    p.op(DVE, lambda e: e.memset(kT[i][0][64:128, :], 0.0), writes=[HB[i]])
                    p.op(DVE, lambda e: e.memset(kT[i][1][0:64, :], 0.0), writes=[HB[i]])
                    p.op(DVE, lambda e: e.memset(vS[i][:, :, 128:129], 1.0), writes=[HB[i]])
                    p.op(DVE, lambda e: e.memset(vS[i][:, :, 129:130], 0.0), writes=[HB[i]])
                ecount = [0]
                aocount = [0]
                nhalf_t = sb(es, "nhalf_t", [128, 1])
                ob = [[sb(es, "ob%d_%d" % (j_, i_), [128, 386]) for i_ in range(4)] for j_ in range(2)]
                OB = [[Buf() for i_ in range(4)] for j_ in range(2)]
                rzs = [sb(es, "rz%d" % i_, [128, 4]) for i_ in range(4)]
                RZS = [Buf() for _ in range(4)]
                o1s = [sb(es, "o1_%d" % i_, [128, 128]) for i_ in range(4)]
                o2s = [sb(es, "o2_%d" % i_, [128, 128]) for i_ in range(4)]
                osqs = [sb(es, "osq_%d" % i_, [128, 128]) for i_ in range(4)]
                ofs = [sb(es, "of_%d" % i_, [128, 128], BF16) for i_ in range(4)]
                O1S = [Buf() for _ in range(4)]
                O2S = [Buf() for _ in range(4)]
                OSQS = [Buf() for _ in range(4)]
                OFS = [Buf() for _ in range(4)]
                NH_ = Buf()
                p.op(DVE, lambda e: e.memset(nhalf_t[:], -0.5), writes=[NH_])
                nhalfT = sb(es, "nhalfT", [128, 512])
                p.op(DVE, lambda e: e.memset(nhalfT[:], -0.5), writes=[NH_])
                subcol = sb(es, "subcol", [128, 1])
                p.dma(SP, lambda q: q.dma_start(out=subcol[:], in_=subw.rearrange("o d -> d o")), writes=[SM])
                p.op(DVE, lambda e: e.tensor_scalar(out=subcol[:], in0=subcol[:], scalar1=float(1.0 - lam_init), scalar2=None, op0=ALU.mult),
                     reads=[SM], writes=[SM])
                eps_t = [[sb(es, "ep%d_%d" % (j_, i_), [128, 512], BF16 if i_ >= 5 else F32) for i_ in range(7)] for j_ in range(2)]
                EPS_B = [[Buf() for i_ in range(7)] for j_ in range(2)]
                qbcount = [0]
                def load_head(h_):
                    hs_ = h_ % 2
                    p.dma(SP, lambda q: q.dma_start(out=qT[hs_][:], in_=QT[h_]), writes=[HB[hs_]])
                    p.dma(SP, lambda q: q.dma_start(out=kT[hs_][0][0:64, :], in_=KT[h_, 0:64, :]), writes=[HB[hs_]])
                    p.dma(SP, lambda q: q.dma_start(out=kT[hs_][1][64:128, :], in_=KT[h_, 64:128, :]), writes=[HB[hs_]])
                    p.dma(SP, lambda q: q.dma_start(out=vS[hs_][:, :, 0:128], in_=VS[h_]), writes=[HB[hs_]])
                pre_jobs = [(m_, e_) for e_ in range(NPRE) for m_ in range(3)]
                PREB = Buf()
                n_qblocks = NH * NG
                per_qb = (len(pre_jobs) + n_qblocks - 1) // n_qblocks

                def prestage(nj):
                    for _ in range(nj):
                        if not pre_jobs:
                            return
                        m_, e_ = pre_jobs.pop(0)
                        src_, dst_ = [(w1, W1B), (w3, W3B), (w2, W2B)][m_]
                        p.dma(POOL, lambda q: q.dma_start(out=dst_[e_], in_=src_[e_], max_dma_last_dim=8192), writes=[])
                load_head(0)
                for h in range(NH):
                    hs = h % 2
                    if h + 1 < NH:
                        load_head(h + 1)
                    for qb in range(NG):
                        nkt = NT + 4 * qb + 4

                        def geom(kt):
                            dj = kt - (NT + 4 * qb)
                            lo = 128 * dj if dj > 0 else 0
                            return dj, lo, 512 - lo

                        def emit_qk(kt, c):
                            dj, lo, n = geom(kt)
                            sbk = c + 6 * (kt % 2)
                            p.op(PE, lambda e: e.matmul(banks[sbk][:, 0:n], kT[hs][c][:, kt * 128:(kt + 1) * 128],
                                                        qT[hs][:, qb * 512 + lo:qb * 512 + 512], start=True, stop=True),
                                 reads=[HB[hs]], writes=[BK[sbk]])
                            es_ = ecount[0] % NER
                            ecount[0] += 1
                            p.op(ACT, lambda e: e.activation(out=et[es_][:, 0:n], in_=banks[sbk][:, 0:n], func=AF.Exp,
                                                             bias=kbt[:, kt:kt + 1], scale=0.125),
                                 reads=[BK[sbk], SM], writes=[ET[es_]])
                            if dj >= 0:
                                p.op(POOL, lambda e: e.tensor_tensor(out=et[es_][:, 0:128], in0=et[es_][:, 0:128], in1=trib[:], op=ALU.mult),
                                     reads=[ET[es_], TRIB], writes=[ET[es_]])
                            return es_

                        def emit_pv(kt, c, es_):
                            dj, lo, n = geom(kt)

                            def fpv(e):
                                e.matmul(banks[2 + c][:, lo:512], vS[hs][:, kt, 0:128], et[es_][:, 0:n], start=(kt == 0), stop=(kt == nkt - 1))
                                return e.matmul(banks[4 + c][:, lo:512], onesb[:], et[es_][:, 0:n], start=(kt == 0), stop=(kt == nkt - 1))
                            p.op(PE, fpv, reads=[ET[es_], HB[hs], ONESB], writes=[BK[2 + c], BK[4 + c]])
                        slots = {}
                        for kt0 in range(2):
                            for c in range(2):
                                slots[(kt0, c)] = emit_qk(kt0, c)
                        prestage(per_qb)
                        for kt in range(nkt):
                            for c in range(2):
                                emit_pv(kt, c, slots.pop((kt, c)))
                                if kt + 2 < nkt:
                                    slots[(kt + 2, c)] = emit_qk(kt + 2, c)
                        par = qbcount[0] % 2
                        qbcount[0] += 1
                        zA, zB, tA, tB, vv, osqb, aot = eps_t[par]
                        ZA, ZB, TA, TB, VV, OSQB, AOT = EPS_B[par]
                        p.op(DVE, lambda e: e.reciprocal(out=zA[:], in_=banks[4][:]), reads=[BK[4]], writes=[ZA])
                        p.op(DVE, lambda e: e.reciprocal(out=zB[:], in_=banks[5][:]), reads=[BK[5]], writes=[ZB])
                        p.op(DVE, lambda e: e.tensor_tensor(out=tA[:], in0=banks[2][:], in1=zA[:], op=ALU.mult), reads=[BK[2], ZA], writes=[TA])
                        p.op(DVE, lambda e: e.tensor_tensor(out=tB[:], in0=banks[3][:], in1=zB[:], op=ALU.mult), reads=[BK[3], ZB], writes=[TB])
                        p.op(DVE, lambda e: e.scalar_tensor_tensor(out=tA[:], in0=tB[:], scalar=nlam[:, 0:1], in1=tA[:], op0=ALU.mult, op1=ALU.add),
                             reads=[TB, NLAM, TA], writes=[TA])
                        p.op(DVE, lambda e: e.tensor_tensor(out=osqb[:], in0=tA[:], in1=tA[:], op=ALU.mult), reads=[TA], writes=[OSQB])
                        p.op(PE, lambda e: e.matmul(banks[6][:], meanb[:], osqb[:], start=True, stop=True), reads=[MEANB, OSQB], writes=[BK[6]])
                        p.op(ACT, lambda e: e.activation(out=vv[:], in_=banks[6][:], func=AF.Ln, bias=epss[:], scale=1.0), reads=[BK[6], EPS], writes=[VV])
                        p.op(ACT, lambda e: e.activation(out=vv[:], in_=vv[:], func=AF.Exp, scale=-0.5), reads=[VV], writes=[VV])
                        p.op(DVE, lambda e: e.scalar_tensor_tensor(out=aot[:], in0=tA[:], scalar=subcol[:, 0:1], in1=vv[:], op0=ALU.mult, op1=ALU.mult),
                             reads=[TA, SM, VV], writes=[AOT])
                        p.dma(SP, lambda q: q.dma_start(out=MIXT[CW + h * 128:CW + (h + 1) * 128, qb * 512:(qb + 1) * 512], in_=aot[:]),
                              reads=[AOT])
                p.barrier()
            if stop == 'C':
                raise _Stop()

            with ExitStack() as es:
                GT = 512
                NTG = GT // 128
                mixT = sb(es, "mixT", [128, KC, GT], BF16)
                MX = Buf()
                NWO = 2
                wo = [sb(es, "wo%d" % i, [128, KC, 256], BF16) for i in range(NWO)]
                WO = [Buf() for _ in range(NWO)]
                ht = [sb(es, "ht%d" % i, [128, D]) for i in range(NTG)]
                HT = [Buf() for _ in range(NTG)]
                ln2t = sb(es, "ln2t", [128, D])
                hn2 = sb(es, "hn2", [128, D])
                HN2B = Buf()
                hn2b = sb(es, "hn2b", [128, D], BF16)
                HN2BB = Buf()
                hn2T = sb(es, "hn2T", [128, KC, 128])
                HN2T = Buf()
                wrt = sb(es, "wrt", [128, KC, NR])
                brt = sb(es, "brt", [128, NR])
                SM = Buf()
                st = [sb(es, "sst%d" % i, [128, 1]) for i in range(3)]
                ST = [Buf() for _ in range(3)]
                lg = sb(es, "lg", [128, NR])
                LG = Buf()
                sc = sb(es, "sc", [128, 16])
                SC = Buf()
                ohg = sb(es, "ohg", [128, NGRP])
                exg = sb(es, "exg", [128, NGRP])
                tmp = sb(es, "tmp", [128, NEXP])
                ein = sb(es, "ein", [128, EPG])
                e2 = sb(es, "e2", [128, EPG])
                oh1 = sb(es, "oh1", [128, EPG])
                oh2 = sb(es, "oh2", [128, EPG])
                A1 = sb(es, "A1", [128, NEXP])
                A2 = sb(es, "A2", [128, NEXP])
                Ab = sb(es, "Ab", [128, NEXP], BF16)
                cum = sb(es, "cum", [128, NEXP])
                slot = sb(es, "slot", [128, NEXP])
                tokid = sb(es, "tokid", [128, 16], I32)
                tokf = sb(es, "tokf", [128, 16])
                rt = sb(es, "rtt", [128, 8])
                sinit = sb(es, "sinit", [128, 16], I32)
                RB = Buf()
                CUM = Buf()
                TOK = Buf()
                ZR = Buf()
                HN2D = Buf()
                SLOTD = Buf()
                p.dma(SP, lambda q: q.dma_start(out=ln2t[:], in_=ln2w.partition_broadcast(128)), writes=[SM])
                p.dma(SP, lambda q: q.dma_start(out=wrt[:], in_=wr.rearrange("(k p) n -> p k n", p=128)), writes=[SM])
                p.dma(SP, lambda q: q.dma_start(out=brt[:], in_=br.partition_broadcast(128)), writes=[SM])
                p.op(DVE, lambda e: e.memset(cum[:], 0.0), writes=[CUM])
                p.op(DVE, lambda e: e.memset(hn2b[:], 0.0), writes=[HN2BB])
                p.dma(SP, lambda q: q.dma_start(out=HN2[T:T + 128, :], in_=hn2b[:]), reads=[HN2BB], writes=[HN2D])
                p.op(POOL, lambda e: e.iota(sinit[:], pattern=[[0, 16]], base=T, channel_multiplier=0), writes=[TOK])
                for e_ in range(NEXP):
                    p.dma(SP, lambda q: q.dma_start(out=SLOT[e_ * CAP:(e_ + 1) * CAP, :], in_=sinit[:]), reads=[TOK], writes=[SLOTD])
                wocount = [0]
                deferred = []
                tokall = sb(es, "tokall", [128, NT, 16], I32)
                TOKA = Buf()
                for ti_ in range(NT):
                    p.op(POOL, lambda e: e.iota(tokall[:, ti_, :], pattern=[[0, 16]], base=ti_ * 128, channel_multiplier=1), writes=[TOKA])
                for gi in range(T // GT):
                    p.dma(SP, lambda q: q.dma_start(out=mixT[:], in_=MIXT[:, gi * GT:(gi + 1) * GT].rearrange("(k p) n -> p k n", p=128)),
                          writes=[MX])
                    for i in range(NTG):
                        r0 = gi * GT + i * 128
                        p.dma(SP, lambda q: q.dma_start(out=ht[i][:], in_=xo[r0:r0 + 128, :]), writes=[HT[i]])
                    for cb in range(D // 256):
                        s = wocount[0] % NWO
                        wocount[0] += 1
                        src = w_out[:, cb * 256:(cb + 1) * 256].rearrange("(k p) n -> p k n", p=128)
                        p.dma(POOL, lambda q: q.dma_start(out=wo[s][:], in_=src), writes=[WO[s]])
                        for i in range(NTG):
                            bk = i

                            def fo(e):
                                r = None
                                for kc in range(KC):
                                    r = e.matmul(banks[bk][:, 0:256], mixT[:, kc, i * 128:(i + 1) * 128], wo[s][:, kc, :],
                                                 start=(kc == 0), stop=(kc == KC - 1))
                                return r
                            p.op(PE, fo, reads=[MX, WO[s]], writes=[BK[bk]])
                            p.op(DVE, lambda e: e.tensor_tensor(out=ht[i][:, cb * 256:(cb + 1) * 256], in0=banks[bk][:, 0:256],
                                                                in1=ht[i][:, cb * 256:(cb + 1) * 256], op=ALU.add),
                                 reads=[BK[bk], HT[i]], writes=[HT[i]])
                    for i in range(NTG):
                        ti = gi * NTG + i
                        r0 = ti * 128
                        p.dma(SP, lambda q: q.dma_start(out=H[r0:r0 + 128, :], in_=ht[i][:]), reads=[HT[i]])
                        p.op(ACT, lambda e: e.activation(out=hn2b[:], in_=ht[i][:], func=AF.Square, accum_out=st[0][:]),
                             reads=[HT[i]], writes=[HN2BB, ST[0]])
                        p.op(ACT, lambda e: e.activation(out=st[1][:], in_=st[0][:], func=AF.Sqrt, bias=epsn[:], scale=1.0 / D),
                             reads=[ST[0], EPS], writes=[ST[1]])
                        p.op(DVE, lambda e: e.reciprocal(out=st[2][:], in_=st[1][:]), reads=[ST[1]], writes=[ST[2]])
                        p.op(DVE, lambda e: e.scalar_tensor_tensor(out=hn2[:], in0=ht[i][:], scalar=st[2][:, 0:1], in1=ln2t[:],
                                                                   op0=ALU.mult, op1=ALU.mult), reads=[HT[i], ST[2], SM], writes=[HN2B])
                        p.op(ACT, lambda e: e.activation(out=hn2b[:], in_=hn2[:], func=AF.Copy), reads=[HN2B], writes=[HN2BB])
                        p.dma(SP, lambda q: q.dma_start(out=HN2[r0:r0 + 128, :], in_=hn2b[:]), reads=[HN2BB], writes=[HN2D])
                        for k4 in range(0, KC, 4):
                            bkid = 4 if (k4 // 4) % 2 == 0 else 7

                            def ftr1(e):
                                r = None
                                for kc in range(k4, k4 + 4):
                                    r = e.transpose(banks[bkid][:, (kc - k4) * 128:(kc - k4 + 1) * 128], hn2[:, kc * 128:(kc + 1) * 128], cst[:, C_ID:C_ID + 128])
                                return r
                            p.op(PE, ftr1, reads=[HN2B, CST], writes=[BK[bkid]])
                            if bkid == 4:
                                p.op(ACT, lambda e: e.activation(out=hn2T[:, k4:k4 + 4, :].rearrange("p a b -> p (a b)"), in_=banks[bkid][:], func=AF.Copy),
                                     reads=[BK[bkid]], writes=[HN2T])
                            else:
                                p.op(DVE, lambda e: e.tensor_copy(out=hn2T[:, k4:k4 + 4, :].rearrange("p a b -> p (a b)"), in_=banks[bkid][:]),
                                     reads=[BK[bkid]], writes=[HN2T])

                        def frt(e):
                            r = None
                            for kc in range(KC):
                                r = e.matmul(banks[5][:, 0:NR], hn2T[:, kc, :], wrt[:, kc, :], start=(kc == 0), stop=(kc == KC - 1))
                            return r
                        p.op(PE, frt, reads=[HN2T, SM], writes=[BK[5]])
                        R = [RB]
                        dv = lambda fn, rd=(), wr_=(): p.op(DVE, fn, reads=list(rd) + R, writes=list(wr_) + R)
                        dv(lambda e: e.tensor_tensor(out=lg[:], in0=banks[5][:, 0:NR], in1=brt[:], op=ALU.add), rd=[BK[5], SM])
                        dv(lambda e: e.tensor_reduce(out=sc[:, 0:1], in_=lg[:, 0:NGRP], axis=AX.X, op=ALU.max))
                        dv(lambda e: e.tensor_scalar(out=ohg[:], in0=lg[:, 0:NGRP], scalar1=sc[:, 0:1], scalar2=None, op0=ALU.is_equal))
                        dv(lambda e: e.tensor_scalar(out=sc[:, 1:2], in0=sc[:, 0:1], scalar1=-1.0, scalar2=None, op0=ALU.mult))
                        p.op(ACT, lambda e: e.activation(out=exg[:], in_=lg[:, 0:NGRP], func=AF.Exp, bias=sc[:, 1:2], scale=1.0, accum_out=sc[:, 2:3]),
                             reads=R, writes=R)
                        dv(lambda e: e.reciprocal(out=sc[:, 3:4], in_=sc[:, 2:3]))
                        le3 = lg[:, NGRP:NR].rearrange("p (g j) -> p g j", g=NGRP)
                        tmp3 = tmp[:].rearrange("p (g j) -> p g j", g=NGRP)
                        dv(lambda e: e.tensor_tensor(out=tmp3, in0=le3, in1=ohg[:].unsqueeze(2).to_broadcast([128, NGRP, EPG]), op=ALU.mult))
                        dv(lambda e: e.tensor_reduce(out=ein[:], in_=tmp[:].rearrange("p (g j) -> p j g", g=NGRP), axis=AX.X, op=ALU.add))
                        dv(lambda e: e.tensor_reduce(out=sc[:, 4:5], in_=ein[:], axis=AX.X, op=ALU.max))
                        dv(lambda e: e.tensor_scalar(out=oh1[:], in0=ein[:], scalar1=sc[:, 4:5], scalar2=None, op0=ALU.is_equal))
                        dv(lambda e: e.scalar_tensor_tensor(out=e2[:], in0=oh1[:], scalar=-1e30, in1=ein[:], op0=ALU.mult, op1=ALU.add))
                        dv(lambda e: e.tensor_reduce(out=sc[:, 5:6], in_=e2[:], axis=AX.X, op=ALU.max))
                        dv(lambda e: e.tensor_scalar(out=oh2[:], in0=e2[:], scalar1=sc[:, 5:6], scalar2=None, op0=ALU.is_equal))
                        dv(lambda e: e.tensor_tensor(out=sc[:, 6:7], in0=sc[:, 5:6], in1=sc[:, 4:5], op=ALU.subtract))
                        p.op(ACT, lambda e: e.activation(out=sc[:, 7:8], in_=sc[:, 6:7], func=AF.Exp), reads=R, writes=R)
                        dv(lambda e: e.tensor_scalar(out=sc[:, 8:9], in0=sc[:, 7:8], scalar1=1.0, scalar2=None, op0=ALU.add))
                        dv(lambda e: e.reciprocal(out=sc[:, 9:10], in_=sc[:, 8:9]))
                        dv(lambda e: e.tensor_tensor(out=sc[:, 10:11], in0=sc[:, 9:10], in1=sc[:, 7:8], op=ALU.mult))
                        dv(lambda e: e.tensor_tensor(out=gates[:, ti, 0:1], in0=sc[:, 9:10], in1=sc[:, 3:4], op=ALU.mult), wr_=[GATES])
                        dv(lambda e: e.tensor_tensor(out=gates[:, ti, 1:2], in0=sc[:, 10:11], in1=sc[:, 3:4], op=ALU.mult), wr_=[GATES])
                        A13 = A1[:].rearrange("p (g j) -> p g j", g=NGRP)
                        A23 = A2[:].rearrange("p (g j) -> p g j", g=NGRP)
                        ohgb = ohg[:].unsqueeze(2).to_broadcast([128, NGRP, EPG])
                        dv(lambda e: e.tensor_tensor(out=A13, in0=ohgb, in1=oh1[:].unsqueeze(1).to_broadcast([128, NGRP, EPG]), op=ALU.mult))
                        dv(lambda e: e.tensor_tensor(out=A23, in0=ohgb, in1=oh2[:].unsqueeze(1).to_broadcast([128, NGRP, EPG]), op=ALU.mult))
                        dv(lambda e: e.tensor_tensor(out=Ab[:], in0=A1[:], in1=A2[:], op=ALU.add))
                        p.op(PE, lambda e: e.matmul(banks[6][:, 0:NEXP], usb[:], Ab[:], start=True, stop=True), reads=[USB] + R, writes=[BK[6]])
                        p.op(PE, lambda e: e.matmul(banks[7][:, 0:NEXP], onesb[:], Ab[:], start=True, stop=True), reads=[ONESB] + R, writes=[BK[7]])
                        dv(lambda e: e.tensor_tensor(out=slot[:], in0=banks[6][:, 0:NEXP], in1=cum[:], op=ALU.add), rd=[BK[6], CUM])
                        dv(lambda e: e.tensor_tensor(out=slot[:], in0=slot[:], in1=cst[:, C_EBASE:C_EBASE + NEXP], op=ALU.add), rd=[CST])
                        dv(lambda e: e.tensor_tensor(out=cum[:], in0=banks[7][:, 0:NEXP], in1=cum[:], op=ALU.add), rd=[BK[7], CUM], wr_=[CUM])
                        dv(lambda e: e.tensor_tensor(out=tmp[:], in0=A1[:], in1=slot[:], op=ALU.mult))
                        dv(lambda e: e.tensor_reduce(out=sc[:, 11:12], in_=tmp[:], axis=AX.X, op=ALU.add))
                        dv(lambda e: e.tensor_tensor(out=tmp[:], in0=A2[:], in1=slot[:], op=ALU.mult))
                        dv(lambda e: e.tensor_reduce(out=sc[:, 12:13], in_=tmp[:], axis=AX.X, op=ALU.add))
                        dv(lambda e: e.tensor_copy(out=dests[:, ti, :], in_=sc[:, 11:13]), wr_=[DESTS])
                        deferred.append(ti)
                        if dbg:
                            dv(lambda e: e.tensor_copy(out=rt[:, 0:2], in_=sc[:, 11:13]))
                            dv(lambda e: e.tensor_copy(out=rt[:, 2:4], in_=gates[:, ti, :]))
                            dv(lambda e: e.tensor_copy(out=rt[:, 4:8], in_=lg[:, 0:4]))
                            p.dma(SP, lambda q: q.dma_start(out=RT[r0:r0 + 128, :], in_=rt[:]), reads=R)
                for ti_ in deferred:
                    for k_ in range(2):
                        p.dma(POOL, lambda q: q.indirect_dma_start(out=SLOT, out_offset=bass.IndirectOffsetOnAxis(dests[:, ti_, k_:k_ + 1], 0),
                                                                   in_=tokall[:, ti_, :], in_offset=None),
                              reads=[TOKA, DESTS], writes=[SLOTD])
                p.barrier()
            if stop == 'D':
                raise _Stop()

            with ExitStack() as es:
                KB = min(8, KC)
                NKB = KC // KB
                CB2 = min(1024, D)
                NCB2 = D // CB2
                idx = [sb(es, "idx%d" % i, [128, 16], I32) for i in range(2)]
                IDX = [Buf() for _ in range(2)]
                xg = [sb(es, "xg%d" % i, [128, D], BF16) for i in range(2)]
                XG = [Buf() for _ in range(2)]
                xgT = sb(es, "xgT", [128, KC, 128], BF16)
                XGT = Buf()
                NWE = 5
                wa = [sb(es, "wa%d" % i, [128, KB * DE], BF16) for i in range(NWE)]
                WA = [Buf() for _ in range(NWE)]
                assert KB * DE >= HC * CB2
                sa = sb(es, "sa", [128, DE])
                SA = Buf()
                hid = sb(es, "hid", [128, DE], BF16)
                HID = Buf()
                hidT = sb(es, "hidT", [128, HC, 128], BF16)
                HIDT = Buf()
                ysb = [sb(es, "ysb%d" % i, [128, CB2]) for i in range(2)]
                YSB = [Buf() for _ in range(2)]
                YD = Buf()
                wcnt = [0]
                ycnt = [0]
                nhalf = max(1, DE // 512)
                hw_ = min(512, DE)

                def load_w(src3):
                    s = wcnt[0] % NWE
                    wcnt[0] += 1
                    kdim, ncol = src3.shape[1], src3.shape[2]
                    dst = wa[s][:, 0:kdim * ncol].rearrange("p (k n) -> p k n", k=kdim)
                    p.dma(POOL, lambda q: q.dma_start(out=dst, in_=src3), writes=[WA[s]])
                    return s, dst

                def prefetch(e_):
                    s = e_ % 2
                    p.dma(SP, lambda q: q.dma_start(out=idx[s][:], in_=SLOT[e_ * CAP:(e_ + 1) * CAP, :]), writes=[IDX[s]])
                    p.dma(POOL, lambda q: q.indirect_dma_start(out=xg[s][:], out_offset=None, in_=HN2,
                                                               in_offset=bass.IndirectOffsetOnAxis(idx[s][:, 0:1], 0)),
                          reads=[IDX[s]], writes=[XG[s]])
                prefetch(0)
                tbt = banks[6][:].bitcast(BF16)
                tbt2 = banks[7][:].bitcast(BF16)
                for e_ in range(NEXP):
                    s = e_ % 2
                    if e_ + 1 < NEXP:
                        prefetch(e_ + 1)
                    for k4 in range(0, KC, 4):
                        nk = min(4, KC - k4)
                        tbk = tbt if (k4 // 4) % 2 == 0 else tbt2
                        bki = 6 if (k4 // 4) % 2 == 0 else 7

                        def ftx(e):
                            r = None
                            for kc in range(k4, k4 + nk):
                                r = e.transpose(tbk[:, (kc - k4) * 128:(kc - k4 + 1) * 128], xg[s][:, kc * 128:(kc + 1) * 128], identb[:])
                            return r
                        p.op(PE, ftx, reads=[XG[s], IDB], writes=[BK[bki]])
                        eng = ACT if (k4 // 4) % 2 == 0 else DVE
                        if eng == ACT:
                            p.op(ACT, lambda e: e.activation(out=xgT[:, k4:k4 + nk, :].rearrange("p a b -> p (a b)"), in_=tbk[:, 0:nk * 128], func=AF.Copy),
                                 reads=[BK[bki]], writes=[XGT])
                        else:
                            p.op(DVE, lambda e: e.tensor_copy(out=xgT[:, k4:k4 + nk, :].rearrange("p a b -> p (a b)"), in_=tbk[:, 0:nk * 128]),
                                 reads=[BK[bki]], writes=[XGT])
                    for wi, wsrc in enumerate((w1, w3)):
                        for kb_ in range(NKB):
                            wsrc_ = (W1B, W3B)[wi] if e_ < NPRE else wsrc
                            src3 = wsrc_[e_, kb_ * KB * 128:(kb_ + 1) * KB * 128, :].rearrange("(k p) n -> p k n", p=128)
                            ws, wv = load_w(src3)

                            def fw(e):
                                r = None
                                for kc in range(KB):
                                    for hf in range(nhalf):
                                        r = e.matmul(banks[wi * 2 + hf][:, 0:hw_], xgT[:, kb_ * KB + kc, :], wv[:, kc, hf * hw_:(hf + 1) * hw_],
                                                     start=(kb_ == 0 and kc == 0), stop=(kb_ == NKB - 1 and kc == KB - 1))
                                return r
                            p.op(PE, fw, reads=[XGT, WA[ws]], writes=[BK[wi * 2 + hf_] for hf_ in range(nhalf)])
                    for hf in range(nhalf):
                        p.op(ACT, lambda e: e.activation(out=sa[:, hf * hw_:(hf + 1) * hw_], in_=banks[hf][:, 0:hw_], func=AF.Silu),
                             reads=[BK[hf]], writes=[SA])
                        p.op(DVE, lambda e: e.tensor_tensor(out=hid[:, hf * hw_:(hf + 1) * hw_], in0=banks[2 + hf][:, 0:hw_], in1=sa[:, hf * hw_:(hf + 1) * hw_],
                                                            op=ALU.mult), reads=[BK[2 + hf], SA], writes=[HID])
                    for k4 in range(0, HC, 4):
                        nk = min(4, HC - k4)

                        def fth(e):
                            r = None
                            for kc in range(k4, k4 + nk):
                                r = e.transpose(tbt[:, (kc - k4) * 128:(kc - k4 + 1) * 128], hid[:, kc * 128:(kc + 1) * 128], identb[:])
                            return r
                        p.op(PE, fth, reads=[HID, IDB], writes=[BK[6]])
                        p.op(ACT, lambda e: e.activation(out=hidT[:, k4:k4 + nk, :].rearrange("p a b -> p (a b)"), in_=tbt[:, 0:nk * 128], func=AF.Copy),
                             reads=[BK[6]], writes=[HIDT])
                    for cb in range(NCB2):
                        src3 = (W2B if e_ < NPRE else w2)[e_, :, cb * CB2:(cb + 1) * CB2].rearrange("(k p) n -> p k n", p=128)
                        ws, wv = load_w(src3)
                        ys = ycnt[0] % 2
                        ycnt[0] += 1
                        for hf in range(CB2 // 512):
                            bk = 4 + hf

                            def fy(e):
                                r = None
                                for kc in range(HC):
                                    r = e.matmul(banks[bk][:], hidT[:, kc, :], wv[:, kc, hf * 512:(hf + 1) * 512], start=(kc == 0), stop=(kc == HC - 1))
                                return r
                            p.op(PE, fy, reads=[HIDT, WA[ws]], writes=[BK[bk]])
                            if hf % 2 == 0:
                                p.op(ACT, lambda e: e.activation(out=ysb[ys][:, hf * 512:(hf + 1) * 512], in_=banks[bk][:], func=AF.Copy),
                                     reads=[BK[bk]], writes=[YSB[ys]])
                            else:
                                p.op(DVE, lambda e: e.tensor_copy(out=ysb[ys][:, hf * 512:(hf + 1) * 512], in_=banks[bk][:]),
                                     reads=[BK[bk]], writes=[YSB[ys]])
                        p.dma(SP, lambda q: q.dma_start(out=Y[e_ * CAP:(e_ + 1) * CAP, cb * CB2:(cb + 1) * CB2], in_=ysb[ys][:]),
                              reads=[YSB[ys]], writes=[YD])
                p.barrier()
            if stop == 'F':
                raise _Stop()

            with ExitStack() as es:
                hx = [sb(es, "hx%d" % i, [128, D]) for i in range(2)]
                y1 = [sb(es, "y1_%d" % i, [128, D]) for i in range(2)]
                y2 = [sb(es, "y2_%d" % i, [128, D]) for i in range(2)]
                HX = [Buf() for _ in range(2)]
                Y1 = [Buf() for _ in range(2)]
                Y2 = [Buf() for _ in range(2)]
                lnft = sb(es, "lnft", [128, D])
                junk = sb(es, "junk3", [128, D], BF16)
                JUNK = Buf()
                SM = Buf()
                st = [sb(es, "gst%d" % i, [128, 1]) for i in range(3)]
                ST = [Buf() for _ in range(3)]
                p.dma(SP, lambda q: q.dma_start(out=lnft[:], in_=lnfw.partition_broadcast(128)), writes=[SM])
                for ti in range(NT):
                    s = ti % 2
                    r0 = ti * 128
                    p.dma(SP, lambda q: q.dma_start(out=hx[s][:], in_=H[r0:r0 + 128, :]), writes=[HX[s]])
                    p.dma(POOL, lambda q: q.indirect_dma_start(out=y1[s][:], out_offset=None, in_=Y,
                                                               in_offset=bass.IndirectOffsetOnAxis(dests[:, ti, 0:1], 0)),
                          reads=[DESTS], writes=[Y1[s]])
                    p.dma(POOL, lambda q: q.indirect_dma_start(out=y2[s][:], out_offset=None, in_=Y,
                                                               in_offset=bass.IndirectOffsetOnAxis(dests[:, ti, 1:2], 0)),
                          reads=[DESTS], writes=[Y2[s]])
                    p.op(DVE, lambda e: e.scalar_tensor_tensor(out=hx[s][:], in0=y1[s][:], scalar=gates[:, ti, 0:1], in1=hx[s][:],
                                                               op0=ALU.mult, op1=ALU.add), reads=[Y1[s], GATES, HX[s]], writes=[HX[s]])
                    p.op(DVE, lambda e: e.scalar_tensor_tensor(out=hx[s][:], in0=y2[s][:], scalar=gates[:, ti, 1:2], in1=hx[s][:],
                                                               op0=ALU.mult, op1=ALU.add), reads=[Y2[s], GATES, HX[s]], writes=[HX[s]])
                    p.op(ACT, lambda e: e.activation(out=junk[:], in_=hx[s][:], func=AF.Square, accum_out=st[0][:]),
                         reads=[HX[s]], writes=[JUNK, ST[0]])
                    p.op(ACT, lambda e: e.activation(out=st[1][:], in_=st[0][:], func=AF.Sqrt, bias=epsn[:], scale=1.0 / D),
                         reads=[ST[0], EPS], writes=[ST[1]])
                    p.op(DVE, lambda e: e.reciprocal(out=st[2][:], in_=st[1][:]), reads=[ST[1]], writes=[ST[2]])
                    p.op(DVE, lambda e: e.scalar_tensor_tensor(out=y1[s][:], in0=hx[s][:], scalar=st[2][:, 0:1], in1=lnft[:],
                                                               op0=ALU.mult, op1=ALU.mult), reads=[HX[s], ST[2], SM], writes=[Y1[s]])
                    p.dma(SP, lambda q: q.dma_start(out=out[r0:r0 + 128, :], in_=y1[s][:]), reads=[Y1[s]])
        except _Stop:
            pass
        print('total ops', p.nops)
        p.wait_all(SP)
    return nc


def make_in_maps(cfg, inputs):
    f = lambda a: np.ascontiguousarray(np.asarray(a))
    x = f(inputs["x"])
    positions = f(inputs["positions"]).astype(np.int32)
    T, D, KC, NCB = cfg.T, cfg.D, cfg.KC, cfg.NCB
    l = 0
    ln1w = f(inputs["ln1_w"][l]).reshape(KC, 128).T.copy()
    w_in = f(inputs["w_in"][l])
    convw = f(inputs["conv_w"][l]).reshape(3, NCB, 128).transpose(2, 1, 0).reshape(128, NCB * 3).copy()
    cnw = f(inputs["conv_norm_w"][l]).reshape(NCB, 128).T.copy()
    lam4 = np.concatenate([f(inputs["lam_q1"][l]), f(inputs["lam_k1"][l]), f(inputs["lam_q2"][l]), f(inputs["lam_k2"][l])]).reshape(1, 256)
    subw = f(inputs["subln_w"][l]).reshape(1, 128)
    w_out = f(inputs["w_out"][l])
    ln2w = f(inputs["ln2_w"][l]).reshape(1, D)
    lnfw = f(inputs["lnf_w"]).reshape(1, D)
    wr = np.concatenate([f(inputs["w_router_group"][l]), f(inputs["w_router_expert"][l])], axis=1)
    br = np.concatenate([f(inputs["b_router_group"][l]), f(inputs["b_router_expert"][l])]).reshape(1, cfg.NR)
    w1 = f(inputs["w1"][l])
    w3 = f(inputs["w3"][l])
    w2 = f(inputs["w2"][l])
    consts = make_consts(cfg)
    NTT = 2 * T // 128
    maps = []
    for c in range(cfg.NC):
        b, half = c // 2, c % 2
        xo = x[b, half * T:(half + 1) * T]
        if half == 1:
            xp = x[b, 0:T]
            ppos = positions[b, 0:T]
        else:
            xp = np.zeros_like(xo)
            ppos = np.zeros(T, np.int32)
        pos = np.concatenate([ppos, positions[b, half * T:(half + 1) * T]]).reshape(1, 2 * T).astype(np.int32)
        kb = np.zeros((128, NTT), np.float32)
        if half == 0:
            kb[:, 0:NTT // 2] = NEG
        maps.append(dict(xo=xo, xp=xp, pos=pos, kb=kb, ln1w=ln1w, w_in=w_in, convw=convw, cnw=cnw, lam4=lam4, subw=subw,
                         w_out=w_out, ln2w=ln2w, lnfw=lnfw, wr=wr, br=br, w1=w1, w3=w3, w2=w2, consts=consts))
    return maps


def kernel(**inputs):
    cfg = Cfg()
    lam_init = 0.8 - 0.6 * math.exp(-0.3 * 0)
    nc = build_nc(cfg, lam_init=lam_init)
    maps = make_in_maps(cfg, inputs)
    res = run_bass_kernel_spmd(nc, maps, core_ids=list(range(cfg.NC)))
    outp = np.zeros((cfg.B, cfg.S, cfg.D), np.float32)
    for c in range(cfg.NC):
        b, half = c // 2, c % 2
        outp[b, half * cfg.T:(half + 1) * cfg.T] = res.results[c]["out"]
    return outp
```
